# Optimizing a Trainium2 kernel written in Bass

```python
import math
import jax, jax.numpy as jnp
from jax import lax
import numpy as np

D_MODEL = 1024
BATCH = 8
SEQ = 2048
DEPTH = 2

CHUNK = 64
HEAD_DIM = 64
N_GROUPS = 4
HEADS_PER_GROUP = D_MODEL // (N_GROUPS * HEAD_DIM)
GROUP_W = HEADS_PER_GROUP * HEAD_DIM
MIX_W = N_GROUPS * GROUP_W
A_LEFT_CHUNKS = 8
REL_CLIP = 128
IDX_HEADS = 8
IDX_DIM = 32
TOPK_CAP = 256
DIFF_DIM = HEAD_DIM // 2
Q_BLOCK = 128
ROPE_THETA = 10000.0
D_FF = -(-8 * D_MODEL // (3 * 256)) * 256
EPS = 1e-6

IN_WIDTHS = (3 * GROUP_W,
             3 * GROUP_W,
             IDX_HEADS * IDX_DIM,
             IDX_DIM,
             IDX_HEADS,
             3 * GROUP_W,
             HEADS_PER_GROUP,
             3 * GROUP_W)
IN_W = sum(IN_WIDTHS)
IN_SPLIT_OFFSETS = tuple(int(o) for o in np.cumsum(IN_WIDTHS)[:-1])

kernel_name = "hybrid_chunk_causal_parallel_heads"


def rmsnorm(x, g):
    xf = x.astype(jnp.float32)
    y = xf * lax.rsqrt(jnp.mean(xf * xf, axis=-1, keepdims=True) + EPS)
    return (y * g.astype(jnp.float32)).astype(x.dtype)


def rope(x, pos):
    d = x.shape[-1]
    inv = ROPE_THETA ** (-jnp.arange(0, d, 2, dtype=jnp.float32) / d)
    ang = pos.astype(jnp.float32)[:, None] * inv[None, :]
    cos = jnp.cos(ang)[:, None, :]
    sin = jnp.sin(ang)[:, None, :]
    xf = x.astype(jnp.float32)
    x1, x2 = xf[..., : d // 2], xf[..., d // 2:]
    return jnp.concatenate([x1 * cos - x2 * sin, x2 * cos + x1 * sin], axis=-1).astype(x.dtype)


def sweep_query_blocks(fn, *qs):
    B, S = qs[0].shape[:2]
    nb = S // Q_BLOCK
    blocks = tuple(jnp.swapaxes(a.reshape(B, nb, Q_BLOCK, *a.shape[2:]), 0, 1) for a in qs)
    out = lax.map(lambda xs: fn(xs[0], *xs[1:]), (jnp.arange(nb), *blocks))
    out = jnp.swapaxes(out, 0, 1)
    return out.reshape(B, S, *out.shape[3:])


def chunk_band_attention(q, k, v, rel_bias):
    B, S, H, d = q.shape
    nc = S // CHUNK
    nband = A_LEFT_CHUNKS + 1
    qc = q.reshape(B, nc, CHUNK, H, d)

    def band(a):
        ac = a.reshape(B, nc, CHUNK, H, d)
        ap = jnp.pad(ac, ((0, 0), (A_LEFT_CHUNKS, 0), (0, 0), (0, 0), (0, 0)))
        return jnp.concatenate([ap[:, j:j + nc] for j in range(nband)], axis=2)

    kb, vb = band(k), band(v)
    s = jnp.einsum('bcihd,bcjhd->bhcij', qc, kb).astype(jnp.float32) * (d ** -0.5)
    i_pos = jnp.arange(CHUNK)[:, None]
    p_pos = jnp.arange(nband * CHUNK)[None, :]
    rel = A_LEFT_CHUNKS * CHUNK + i_pos - p_pos
    bias = rel_bias[:, jnp.clip(rel, -REL_CLIP, REL_CLIP) + REL_CLIP].astype(jnp.float32)
    key_chunk = jnp.arange(nc)[:, None] - A_LEFT_CHUNKS + p_pos // CHUNK
    valid = (key_chunk >= 0)[None, None, :, None, :]
    s = jnp.where(valid, s + bias[None, :, None], -jnp.inf)
    p = jax.nn.softmax(s, axis=-1).astype(v.dtype)
    o = jnp.einsum('bhcij,bcjhd->bcihd', p, vb)
    return o.reshape(B, S, H * d)


def dsa_sparse_attention(q, k, v, q_idx, k_idx, w_idx):
    B, S, H, d = q.shape
    n_sel = min(TOPK_CAP, S // 4)
    key_chunk = jnp.arange(S) // CHUNK

    def block(bi, qb, qib, wb):
        q_chunk = (bi * Q_BLOCK + jnp.arange(Q_BLOCK)) // CHUNK
        admiss = key_chunk[None, :] <= q_chunk[:, None]
        sc = jnp.einsum('bqgd,bsd->bqgs', qib, k_idx).astype(jnp.float32) * (IDX_DIM ** -0.5)
        score = jnp.einsum('bqg,bqgs->bqs', wb.astype(jnp.float32) * (IDX_HEADS ** -0.5), jax.nn.relu(sc))
        score = jnp.where(admiss[None], score, -jnp.inf)
        _, sel = lax.top_k(score, n_sel)
        sel_ok = key_chunk[sel] <= q_chunk[None, :, None]
        ks = jax.vmap(lambda kk, ii: kk[ii])(k, sel)
        vs = jax.vmap(lambda vv, ii: vv[ii])(v, sel)
        s = jnp.einsum('bqhd,bqkhd->bhqk', qb, ks).astype(jnp.float32) * (d ** -0.5)
        s = jnp.where(sel_ok[:, None], s, -jnp.inf)
        p = jax.nn.softmax(s, axis=-1).astype(v.dtype)
        return jnp.einsum('bhqk,bqkhd->bqhd', p, vs)

    o = sweep_query_blocks(block, q, q_idx, w_idx)
    return o.reshape(B, S, H * d)


def forgetting_attention(q, k, v, f_logit):
    B, S, H, d = q.shape
    cum = jnp.cumsum(jax.nn.log_sigmoid(f_logit.astype(jnp.float32)), axis=1)
    cum_keys = jnp.swapaxes(cum, 1, 2)
    key_pos = jnp.arange(S)

    def block(bi, qb, cb):
        q_pos = bi * Q_BLOCK + jnp.arange(Q_BLOCK)
        s = jnp.einsum('bqhd,bshd->bhqs', qb, k).astype(jnp.float32) * (d ** -0.5)
        s = s + jnp.swapaxes(cb, 1, 2)[..., None] - cum_keys[:, :, None, :]
        s = jnp.where((key_pos[None, :] <= q_pos[:, None])[None, None], s, -jnp.inf)
        p = jax.nn.softmax(s, axis=-1).astype(v.dtype)
        return jnp.einsum('bhqs,bshd->bqhd', p, v)

    o = sweep_query_blocks(block, q, cum)
    return o.reshape(B, S, H * d)


def differential_attention(q, k, v, lam, subln_g, lam_init):
    B, S, H, d = v.shape
    key_chunk = jnp.arange(S) // CHUNK

    def block(bi, qb):
        q_chunk = (bi * Q_BLOCK + jnp.arange(Q_BLOCK)) // CHUNK
        mask = key_chunk[None, :] <= q_chunk[:, None]
        s = jnp.einsum('bqhrd,bshrd->bhrqs', qb, k).astype(jnp.float32) * (DIFF_DIM ** -0.5)
        s = jnp.where(mask[None, None, None], s, -jnp.inf)
        p = jax.nn.softmax(s, axis=-1)
        a = (p[:, :, 0] - lam * p[:, :, 1]).astype(v.dtype)
        return jnp.einsum('bhqs,bshd->bqhd', a, v)

    o = sweep_query_blocks(block, q)
    o = rmsnorm(o, subln_g) * (1.0 - lam_init)
    return o.reshape(B, S, H * d)


def setup_inputs(seed: int = 0) -> dict:
    key = jax.random.key(seed)
    ks = jax.random.split(key, 16)
    f32 = jnp.float32
    L = DEPTH
    nrm = lambda k, shape, scale: jax.random.normal(k, shape, f32) * scale
    return {
        "x": nrm(ks[0], (BATCH, SEQ, D_MODEL), 1.0),
        "ln1_g": 1.0 + nrm(ks[1], (L, D_MODEL), 0.02),
        "w_in": nrm(ks[2], (L, D_MODEL, IN_W), D_MODEL ** -0.5),
        "rel_bias": nrm(ks[3], (L, HEADS_PER_GROUP, 2 * REL_CLIP + 1), 0.1),
        "forget_b": 1.0 + 2.0 * jax.random.uniform(ks[4], (L, HEADS_PER_GROUP), f32),
        "lam_q1": nrm(ks[5], (L, DIFF_DIM), 0.1),
        "lam_k1": nrm(ks[6], (L, DIFF_DIM), 0.1),
        "lam_q2": nrm(ks[7], (L, DIFF_DIM), 0.1),
        "lam_k2": nrm(ks[8], (L, DIFF_DIM), 0.1),
        "diff_norm_g": 1.0 + nrm(ks[9], (L, HEAD_DIM), 0.02),
        "w_o": nrm(ks[10], (L, MIX_W, D_MODEL), MIX_W ** -0.5),
        "ln2_g": 1.0 + nrm(ks[11], (L, D_MODEL), 0.02),
        "w_gate": nrm(ks[12], (L, D_MODEL, D_FF), D_MODEL ** -0.5),
        "w_up": nrm(ks[13], (L, D_MODEL, D_FF), D_MODEL ** -0.5),
        "w_down": nrm(ks[14], (L, D_FF, D_MODEL), D_FF ** -0.5),
        "final_g": 1.0 + nrm(ks[15], (D_MODEL,), 0.02),
    }


def reference(x, ln1_g, w_in, rel_bias, forget_b, lam_q1, lam_k1, lam_q2, lam_k2,
              diff_norm_g, w_o, ln2_g, w_gate, w_up, w_down, final_g):
    B, S, _ = x.shape
    H, d = HEADS_PER_GROUP, HEAD_DIM
    pos = jnp.arange(S)
    for l in range(DEPTH):
        lam_init = 0.8 - 0.6 * math.exp(-0.3 * l)
        h = rmsnorm(x, ln1_g[l])
        proj = jnp.einsum('bsd,de->bse', h, w_in[l])
        a_qkv, b_qkv, qi, ki, wi, c_qkv, fg, d_qkv = jnp.split(proj, IN_SPLIT_OFFSETS, axis=-1)

        aq, ak, av = [t[:, :, 0] for t in jnp.split(a_qkv.reshape(B, S, 3, H, d), 3, axis=2)]
        out_a = chunk_band_attention(aq, ak, av, rel_bias[l])

        bq, bk, bv = [t[:, :, 0] for t in jnp.split(b_qkv.reshape(B, S, 3, H, d), 3, axis=2)]
        q_idx = rope(qi.reshape(B, S, IDX_HEADS, IDX_DIM), pos)
        k_idx = rope(ki.reshape(B, S, 1, IDX_DIM), pos)[:, :, 0]
        out_b = dsa_sparse_attention(rope(bq, pos), rope(bk, pos), bv, q_idx, k_idx, wi)

        cq, ck, cv = [t[:, :, 0] for t in jnp.split(c_qkv.reshape(B, S, 3, H, d), 3, axis=2)]
        out_c = forgetting_attention(cq, ck, cv, fg + forget_b[l])

        dq, dk, dv = jnp.split(d_qkv, 3, axis=-1)
        dq = rope(dq.reshape(B, S, 2 * H, DIFF_DIM), pos).reshape(B, S, H, 2, DIFF_DIM)
        dk = rope(dk.reshape(B, S, 2 * H, DIFF_DIM), pos).reshape(B, S, H, 2, DIFF_DIM)
        lam = (jnp.exp(jnp.sum(lam_q1[l].astype(jnp.float32) * lam_k1[l].astype(jnp.float32)))
               - jnp.exp(jnp.sum(lam_q2[l].astype(jnp.float32) * lam_k2[l].astype(jnp.float32)))
               + lam_init)
        out_d = differential_attention(dq, dk, dv.reshape(B, S, H, d), lam, diff_norm_g[l], lam_init)

        mixed = jnp.concatenate([out_a, out_b, out_c, out_d], axis=-1)
        x = x + jnp.einsum('bse,ed->bsd', mixed, w_o[l])

        h2 = rmsnorm(x, ln2_g[l])
        gate = jax.nn.silu(jnp.einsum('bsd,df->bsf', h2, w_gate[l]))
        up = jnp.einsum('bsd,df->bsf', h2, w_up[l])
        x = x + jnp.einsum('bsf,fd->bsd', gate * up, w_down[l])
    return rmsnorm(x, final_g)
```

```python
import math
from contextlib import ExitStack

import numpy as np
import concourse.bass as bass
import concourse.mybir as mybir
from concourse.bass_utils import run_bass_kernel_spmd

F32 = mybir.dt.float32
BF16 = mybir.dt.bfloat16
AF = mybir.ActivationFunctionType
ALU = mybir.AluOpType
AX = mybir.AxisListType

ENGS = ("pe", "act", "dve", "pool", "sp")

SEQ = 2048
DM = 1024
NT = 16
DFF = 2816
NL = 2
EPS = 1e-6
NCORES = 8


class Tok:
    __slots__ = ("name", "w", "r")

    def __init__(self, name=""):
        self.name = name
        self.w = None
        self.r = []


class Op:
    __slots__ = ("eng", "fn", "deps", "signal", "seq", "is_dma", "dsem", "dval")

    def __init__(self, eng, fn, is_dma=False):
        self.eng = eng
        self.fn = fn
        self.deps = []
        self.signal = False
        self.seq = None
        self.is_dma = is_dma
        self.dsem = None
        self.dval = None


class Sched:
    def __init__(self, nc, n_dma_sems=8):
        self.nc = nc
        self.q = {e: [] for e in ENGS}
        self.n_dma_sems = n_dma_sems

    def add(self, eng, fn, reads=(), writes=(), is_dma=False):
        op = Op(eng, fn, is_dma)
        deps = []
        for t in reads:
            if t.w is not None:
                deps.append((t.w, "raw"))
        for t in writes:
            if t.w is not None:
                deps.append((t.w, "waw"))
            for r in t.r:
                deps.append((r, "war"))
        seen = set()
        for d, kind in deps:
            if d is op or id(d) in seen:
                continue
            if d.eng == eng and not d.is_dma and not is_dma:
                if eng in ("pe", "sp"):
                    continue
                if kind != "raw":
                    continue
            seen.add(id(d))
            op.deps.append(d)
            d.signal = True
        for t in reads:
            t.r.append(op)
        for t in writes:
            t.w = op
            t.r = []
        if is_dma:
            op.signal = True
        self.q[eng].append(op)
        return op

    def barrier(self):
        lasts = []
        for e in ENGS:
            ql = self.q[e]
            for o in reversed(ql):
                if not o.is_dma and o.fn is not None:
                    lasts.append(o)
                    break
            cnt = 0
            for o in reversed(ql):
                if o.is_dma:
                    lasts.append(o)
                    cnt += 1
                    if cnt >= self.n_dma_sems:
                        break
        for e in ENGS:
            op = Op(e, None)
            for d in lasts:
                if d.eng == e and not d.is_dma:
                    continue
                op.deps.append(d)
                d.signal = True
            self.q[e].append(op)

    def emit(self, sems, dma_sems):
        nc = self.nc
        for e in ENGS:
            c = 0
            k = 0
            for o in self.q[e]:
                if o.is_dma:
                    o.dsem = dma_sems[e][k % self.n_dma_sems]
                    o.dval = 16 * (k // self.n_dma_sems + 1)
                    k += 1
                elif o.signal:
                    c += 1
                    o.seq = c
        handles = {"pe": "tensor", "act": "scalar", "dve": "vector", "pool": "gpsimd", "sp": "sync"}
        stats = {}
        with nc.Block() as block:
            for e in ENGS:
                ops = self.q[e]
                if not ops:
                    continue

                def body(eng, e=e, ops=ops):
                    waited = {}
                    nw = 0
                    for o in ops:
                        wl = []
                        for d in o.deps:
                            if d.is_dma:
                                wl.append((d.dsem, d.dval))
                            else:
                                wl.append((sems[d.eng], d.seq))
                        if o.is_dma and o.dval > 16:
                            wl.append((o.dsem, o.dval - 16))
                        for sem, val in wl:
                            key = id(sem)
                            if waited.get(key, 0) >= val:
                                continue
                            waited[key] = val
                            eng.wait_ge(sem, val)
                            nw += 1
                        if o.fn is None:
                            continue
                        ins = o.fn(eng)
                        if o.is_dma:
                            ins.then_inc(o.dsem, 16)
                        elif o.signal:
                            ins.then_inc(sems[e], 1)
                    for o in ops[::-1]:
                        if o.is_dma:
                            key = id(o.dsem)
                            if waited.get(key, 0) >= o.dval:
                                continue
                            waited[key] = o.dval
                            eng.wait_ge(o.dsem, o.dval)
                    stats[e] = (len(ops), nw)

                getattr(block, handles[e])(body)
        return stats


A0, B0, QI0, KI0, WI0, C0, FG0, D0 = 0, 768, 1536, 1792, 1824, 1832, 2600, 2604


def _swap(cols, d):
    c = cols.reshape(-1, d)
    h = d // 2
    return np.concatenate([c[:, h:], c[:, :h]], axis=1).reshape(-1)


def _inter(c, d):
    s = _swap(c, d)
    return np.concatenate([c[0:128], s[0:128], c[128:256], s[128:256]])


def _win_units():
    ar = np.arange
    u = {}
    u["A_f0"] = A0 + ar(512)
    u["A_t"] = A0 + 512 + ar(256)
    u["C_f0"] = C0 + ar(512)
    u["C_t"] = np.concatenate([C0 + 512 + ar(256), FG0 + ar(4)])
    u["D_f0"] = _inter(D0 + ar(256), 32)
    u["D_f1"] = _inter(D0 + 256 + ar(256), 32)
    u["D_t"] = D0 + 512 + ar(256)
    u["B_f0"] = _inter(B0 + ar(256), 64)
    u["B_f1"] = _inter(B0 + 256 + ar(256), 64)
    u["B_f2"] = _inter(QI0 + ar(256), 32)
    ki4 = np.tile(KI0 + ar(32), 4)
    u["B_f3"] = np.concatenate([ki4, _swap(ki4, 32)])
    u["B_t"] = np.concatenate([B0 + 512 + ar(256), WI0 + ar(8)])
    offs = {}
    o = 0
    cols = []
    for k, v in u.items():
        offs[k] = (o, len(v))
        o += len(v)
        cols.append(v)
    return offs, np.concatenate(cols)


WIN_OFFS, WIN_COLS = _win_units()
NCOL = len(WIN_COLS)


def _rope_tables(d):
    half = d // 2
    inv = (10000.0 ** (-np.arange(0, d, 2, dtype=np.float32) / np.float32(d))).astype(np.float32)
    pos = np.arange(SEQ, dtype=np.float32)
    ang = (pos[:, None] * inv[None, :]).astype(np.float32)
    cos = np.cos(ang).astype(np.float32).T
    sin = np.sin(ang).astype(np.float32).T
    p = np.arange(128)
    j = p % half
    sign = np.where((p % d) < half, -1.0, 1.0).astype(np.float32)
    cs = np.ascontiguousarray(cos[j])
    ss = np.ascontiguousarray(sin[j] * sign[:, None])
    return cs.astype(np.float32), ss.astype(np.float32)


def build_program(n_layers=NL, mixers="ACDB", do_ffn=True, do_final=True, debug=False, nbis=26):
    nc = bass.Bass("TRN2", target_bir_lowering=False)
    dram = lambda name, shape, dt=F32, kind="ExternalInput": nc.dram_tensor(name, list(shape), dt, kind=kind).ap()
    x_d = dram("x", [SEQ, DM])
    win_d = dram("win", [NL, DM, NCOL])
    wo_d = dram("wo", [NL, DM, DM])
    wg_d = dram("wg", [NL, DM, DFF])
    wu_d = dram("wu", [NL, DM, DFF])
    wd_d = dram("wd", [NL, DFF, DM])
    g1_d = dram("g1", [NL, DM])
    g2_d = dram("g2", [NL, DM])
    gf_d = dram("gf", [1, DM])
    relb_d = dram("relb", [NL, 128, 4 * 2 * 128])
    relc_d = dram("relc", [NL, 4])
    fb_d = dram("fb", [NL, 4])
    lam_d = dram("lamv", [NL, 128])
    dng_d = dram("dng", [NL, 64])
    cs64_d = dram("cs64", [128, SEQ])
    ss64_d = dram("ss64", [128, SEQ])
    cs32_d = dram("cs32", [128, SEQ])
    ss32_d = dram("ss32", [128, SEQ])
    out_d = dram("out", [SEQ, DM], F32, "ExternalOutput")
    xres_d = dram("xres", [SEQ, DM], F32, "Internal")
    if debug:
        dbg_h = dram("dbg_h", [128, 8 * SEQ], BF16, "ExternalOutput")
        dbg_m = dram("dbg_m", [128, 8 * SEQ], BF16, "ExternalOutput")
        dbg_x = dram("dbg_x", [SEQ, DM], F32, "ExternalOutput")

    ARENA = 212736
    with ExitStack() as es:
        arena = es.enter_context(nc.sbuf_tensor("arena", [128, ARENA // 4], F32))
        psb = [es.enter_context(nc.psum_tensor(f"ps{i}", [128, 512], F32)) for i in range(8)]
        sems = {e: es.enter_context(nc.semaphore("s_" + e)) for e in ENGS}
        dsems = {e: [es.enter_context(nc.semaphore(f"d_{e}{i}")) for i in range(8)] for e in ENGS}
        S = Sched(nc)

        def carve(off, shape, dt):
            esz = 4 if dt == F32 else 2
            n = int(np.prod(shape[1:]))
            assert off % 4 == 0 and (n * esz) % 4 == 0, (off, shape)
            a = arena[0:shape[0], off // 4: off // 4 + n * esz // 4]
            if dt != F32:
                a = a.bitcast(dt)
            if len(shape) == 3:
                a = a.rearrange("p (a b) -> p a b", a=shape[1])
            elif len(shape) == 4:
                a = a.rearrange("p (a b c) -> p a b c", a=shape[1], b=shape[2])
            return a

        hT = carve(0, [128, 8, SEQ], BF16)
        mixT = carve(32768, [128, 8, SEQ], BF16)
        U = 65536
        X = carve(U, [128, NT, DM], F32)
        QT = carve(U + 0, [128, 2, SEQ], BF16)
        KT = carve(U + 8192, [128, 2, SEQ], BF16)
        V = carve(U + 16384, [128, NT, 4, 128], BF16)
        qiT = carve(U + 32768, [128, 2, SEQ], BF16)
        kiT = carve(U + 40960, [128, SEQ], BF16)
        wiT = carve(U + 45056, [128, NT, 8], F32)
        fgT = carve(U + 45568, [128, NT, 4], F32)
        cumT = carve(U + 45824, [128, NT, 4], F32)
        cbT = carve(U + 46080, [128, NT, 4], F32)
        gexT = carve(U + 46336, [128, NT, 4], F32)
        biasC = carve(U + 46592, [128, 4, 136], F32)
        W = 131072
        wunit = [carve(W + i * 8192, [128, 8, 512], BF16) for i in range(4)]
        hb = [carve(W + 32768 + i * 2048, [128, DM], BF16) for i in range(2)]
        junk = carve(W + 36864, [128, DM], BF16)
        PT = [carve(W + 38912 + i * 1024, [128, 512], BF16) for i in range(4)] + \
             [carve(U + 62592 + i * 1024, [128, 512], BF16) for i in range(2)]
        tmp = [carve(W + 43008 + i * 2048, [128, 512], F32) for i in range(4)]
        obuf = [carve(W + 43008 + i * 4096, [128, DM], F32) for i in range(2)]
        G = carve(W + 51200, [128, DM], F32)
        ident = carve(W + 55296, [128, 128], BF16)
        cst = carve(W + 55296 + 256, [128, 192], F32)
        relbT = carve(W + 56320, [128, 4, 2, 128], F32)
        TAB = [carve(W + 60416 + i * 8192, [128, SEQ], F32) for i in range(2)]
        small2 = carve(W + 76800, [128, 1024], F32)
        PK = [carve(W + 80896 + i * 256, [128, 128], BF16) for i in range(3)] + [cst[:, 128:192].bitcast(BF16)]
        assert W + 80896 + 768 == ARENA
        T_pk = [Tok() for _ in range(4)]
        scoreb = carve(0, [128, SEQ], F32)
        selb = carve(8192, [128, SEQ], BF16)
        selT = carve(12288, [128, NT, 512], BF16)
        junkb = carve(28672, [128, SEQ], BF16)

        eps_t = cst[:, 0:1]
        relc_t = cst[:, 4:8]
        fb_t = cst[:, 8:12]
        lam_t = cst[:, 12:13]
        nlam_t = cst[:, 13:14]
        lamw = cst[:, 16:48]
        dngT = cst[:, 64:128]
        dngc2 = cst[:, 52:53]
        lamv = small2[:, 0:128]
        tri = small2[:, 128:256]
        mhalf = small2[:, 256:384]
        ones_f = small2[:, 384:512]
        bis = small2[:, 512:640]
        small = small2[:, 640:896]
        identf = tmp[0][:, 0:128]

        T_ps = [Tok(f"ps{i}") for i in range(8)]
        T_hT = [Tok(f"hT{i}") for i in range(NT)]
        T_mix = [[Tok(f"mix{m}{g}") for g in range(4)] for m in range(4)]
        T_X = [Tok(f"X{i}") for i in range(NT)]
        T_QT = [[Tok() for _ in range(4)] for _ in range(2)]
        T_KT = [[Tok() for _ in range(4)] for _ in range(2)]
        T_qi = [[Tok() for _ in range(4)] for _ in range(2)]
        T_ki = [Tok() for _ in range(4)]
        T_V = [Tok() for _ in range(NT)]
        T_wu = [Tok() for _ in range(4)]
        T_hb = [Tok() for _ in range(2)]
        T_PT = [Tok() for _ in range(6)]
        T_tmp = [Tok() for _ in range(4)]
        T_otm = [Tok() for _ in range(2)]
        T_dtmp = Tok()
        T_small = Tok()
        T_G = Tok()
        T_cst = Tok()
        T_tab = Tok()
        T_misc = Tok()
        T_junk = Tok()
        T_ss = [Tok() for _ in range(NT)]
        T_relb = Tok()
        T_score = Tok()
        T_sel = Tok()
        T_selT = [Tok() for _ in range(4)]
        T_bis = Tok()
        T_xres = Tok()
        T_out = Tok()
        T_dbg = Tok()

        def mm(out, lhsT, rhs, start, stop, reads, writes, **kw):
            S.add("pe", lambda e: e.matmul(out, lhsT=lhsT, rhs=rhs, start=start, stop=stop, **kw), reads, writes)

        def tr(out, in_, reads, writes):
            S.add("pe", lambda e: e.transpose(out=out, in_=in_, identity=ident), reads, writes)

        def act(out, in_, func, reads, writes, **kw):
            S.add("act", lambda e: e.activation(out=out, in_=in_, func=func, **kw), reads, writes)

        def ts(eng, out, in0, s1, s2, op0, op1, reads, writes, **kw):
            if op1 is None:
                S.add(eng, lambda e: e.tensor_scalar(out=out, in0=in0, scalar1=s1, scalar2=None, op0=op0, **kw), reads, writes)
            else:
                S.add(eng, lambda e: e.tensor_scalar(out=out, in0=in0, scalar1=s1, scalar2=s2, op0=op0, op1=op1, **kw), reads, writes)

        def tt(eng, out, in0, in1, op, reads, writes):
            S.add(eng, lambda e: e.tensor_tensor(out=out, in0=in0, in1=in1, op=op), reads, writes)

        def stt(out, in0, scalar, in1, op0, op1, reads, writes):
            S.add("dve", lambda e: e.scalar_tensor_tensor(out=out, in0=in0, scalar=scalar, in1=in1, op0=op0, op1=op1), reads, writes)

        def cp(eng, out, in_, reads, writes):
            if eng == "act":
                act(out, in_, AF.Copy, reads, writes)
            else:
                S.add(eng, lambda e: e.tensor_copy(out=out, in_=in_), reads, writes)

        def memset(eng, ap, val, reads, writes):
            S.add(eng, lambda e: e.memset(ap, val), reads, writes)

        def dma(eng, out, in_, reads, writes):
            S.add(eng, lambda e: e.dma_start(out=out, in_=in_), reads, writes, is_dma=True)

        def bcast_rows(src_row_ap, nparts=128):
            return src_row_ap.broadcast_to([nparts, src_row_ap.shape[-1]])

        ps_rr = [0]

        def next_ps():
            i = ps_rr[0] % 8
            ps_rr[0] += 1
            return i

        wlist = []

        def wsrc(d_ap, l, rows0, nrows, c0, ncols):
            return (d_ap[l, rows0:rows0 + nrows, c0:c0 + ncols], nrows // 128, ncols)

        order = [m for m in "ACDB" if m in mixers]
        for l in range(n_layers):
            for m in order:
                names = {"A": ["A_f0", "A_t"], "C": ["C_f0", "C_t"], "D": ["D_f0", "D_f1", "D_t"],
                         "B": ["B_f0", "B_f1", "B_f2", "B_f3", "B_t"]}[m]
                for nm in names:
                    o, n = WIN_OFFS[nm]
                    wlist.append(wsrc(win_d, l, 0, DM, o, n))
            for hf in range(2):
                wlist.append(wsrc(wo_d, l, 0, DM, hf * 512, 512))
            if do_ffn:
                for (f0, nf) in ((0, 1024), (1024, 1024), (2048, 768)):
                    for c0 in range(f0, f0 + nf, 512):
                        nn = min(512, f0 + nf - c0)
                        wlist.append(wsrc(wg_d, l, 0, DM, c0, nn))
                        wlist.append(wsrc(wu_d, l, 0, DM, c0, nn))
                    for hf in range(2):
                        wlist.append(wsrc(wd_d, l, f0, nf, hf * 512, 512))
        wstate = {"next_load": 0, "next_use": 0}

        def w_issue():
            i = wstate["next_load"]
            if i >= len(wlist):
                return
            src, kc, ncols = wlist[i]
            slot = i % 4
            dma("pool", wunit[slot][:, 0:kc, 0:ncols], src.rearrange("(kc p) c -> p kc c", p=128), [], [T_wu[slot]])
            wstate["next_load"] += 1

        def w_get():
            i = wstate["next_use"]
            wstate["next_use"] += 1
            assert i < wstate["next_load"]
            return i % 4

        def w_release():
            w_issue()

        memset("pool", identf, 1.0, [], [T_tmp[0]])
        S.add("pool", lambda e: e.affine_select(out=identf, in_=identf, pattern=[[1, 128]], compare_op=ALU.is_equal,
                                                fill=0.0, base=0, channel_multiplier=-1), [T_tmp[0]], [T_tmp[0]])
        cp("dve", ident, identf, [T_tmp[0]], [T_cst])
        memset("dve", eps_t, EPS, [], [T_cst])
        memset("pool", tri, 1.0, [], [T_misc])
        S.add("pool", lambda e: e.affine_select(out=tri, in_=tri, pattern=[[1, 128]], compare_op=ALU.is_ge,
                                                fill=0.0, base=0, channel_multiplier=-1), [T_misc], [T_misc])
        memset("pool", mhalf, 1.0, [], [T_misc])
        S.add("pool", lambda e: e.affine_select(out=mhalf, in_=mhalf, pattern=[[0, 128]], compare_op=ALU.is_ge,
                                                fill=0.0, base=64, channel_multiplier=-1), [T_misc], [T_misc])
        memset("pool", ones_f, 1.0, [], [T_misc])
        for _ in range(4):
            w_issue()

        def norm_to_hT(l, gsrc):
            dma("sp", G, bcast_rows(gsrc), [], [T_G])
            for tb in range(NT):
                b = tb % 2
                ssq = small[:, tb:tb + 1]
                std = small[:, 16 + tb:17 + tb]
                rstd = small[:, 32 + tb:33 + tb]
                act(junk, X[:, tb, :], AF.Square, [T_X[tb]], [T_junk, T_ss[tb]], accum_out=ssq)
                act(std, ssq, AF.Sqrt, [T_ss[tb], T_cst], [T_ss[tb]], scale=1.0 / DM, bias=eps_t)
                S.add("dve", lambda e, o=rstd, i=std: e.reciprocal(out=o, in_=i), [T_ss[tb]], [T_ss[tb]])
                stt(hb[b], X[:, tb, :], rstd, G, ALU.mult, ALU.mult, [T_X[tb], T_ss[tb], T_G], [T_hb[b]])
                pi = next_ps()
                pT = psb[pi][:, :].bitcast(BF16)
                for kc in range(8):
                    tr(pT[:, kc * 128:(kc + 1) * 128], hb[b][:, kc * 128:(kc + 1) * 128], [T_hb[b], T_cst], [T_ps[pi]])
                cp("act", hT[:, :, tb * 128:(tb + 1) * 128], pT.rearrange("p (a b) -> p a b", a=8), [T_ps[pi]], [T_hT[tb]])

        def proj_feature(slot, nchunks, evac):
            for ci in range(nchunks):
                for tg in range(4):
                    pi = next_ps()
                    for kc in range(8):
                        mm(psb[pi][:, :], wunit[slot][:, kc, ci * 128:(ci + 1) * 128], hT[:, kc, tg * 512:(tg + 1) * 512],
                           kc == 0, kc == 7, [T_wu[slot]] + T_hT[tg * 4:tg * 4 + 4], [T_ps[pi]])
                    evac(ci, tg, pi)

        def proj_feature_pairs(slot, npairs, evac):
            for pj in range(npairs):
                for tg in range(4):
                    pis = []
                    for s in range(2):
                        ci = pj * 2 + s
                        pi = next_ps()
                        pis.append(pi)
                        for kc in range(8):
                            mm(psb[pi][:, :], wunit[slot][:, kc, ci * 128:(ci + 1) * 128], hT[:, kc, tg * 512:(tg + 1) * 512],
                               kc == 0, kc == 7, [T_wu[slot]] + T_hT[tg * 4:tg * 4 + 4], [T_ps[pi]])
                    evac(pj, tg, pis)

        def proj_token(slot, ncols, evac):
            for tb in range(NT):
                pi = next_ps()
                for kc in range(8):
                    mm(psb[pi][:, 0:ncols], hT[:, kc, tb * 128:(tb + 1) * 128], wunit[slot][:, kc, 0:ncols],
                       kc == 0, kc == 7, [T_wu[slot], T_hT[tb]], [T_ps[pi]])
                evac(tb, pi)

        ev_rr = [0]

        def plain_evac(dst_fn, tok_fn):
            def f(ci, tg, pi):
                eng = "act" if ev_rr[0] % 2 == 0 else "dve"
                ev_rr[0] += 1
                cp(eng, dst_fn(ci, tg), psb[pi][:, :], [T_ps[pi]], [tok_fn(ci, tg)])
            return f

        rope_rr = [0]

        def rope_evac(dst_fn, tok_fn):
            def f(pj, tg, pis):
                k = rope_rr[0] % 2
                rope_rr[0] += 1
                t1, t2 = tmp[2 * k], tmp[2 * k + 1]
                tt("dve", t1, psb[pis[0]][:, :], TAB[0][:, tg * 512:(tg + 1) * 512], ALU.mult, [T_ps[pis[0]], T_tab], [T_tmp[2 * k]])
                tt("dve", t2, psb[pis[1]][:, :], TAB[1][:, tg * 512:(tg + 1) * 512], ALU.mult, [T_ps[pis[1]], T_tab], [T_tmp[2 * k + 1]])
                tt("pool", dst_fn(pj, tg), t1, t2, ALU.add, [T_tmp[2 * k], T_tmp[2 * k + 1]], [tok_fn(pj, tg)])
            return f

        def v_evac(extra=None):
            def f(tb, pi):
                eng = "act" if tb % 2 == 0 else "dve"
                cp(eng, V[:, tb, :, 0:64], psb[pi][:, 0:256].rearrange("p (h d) -> p h d", h=4), [T_ps[pi]], [T_V[tb]])
                if extra is not None:
                    extra(tb, pi)
            return f

        def load_tables(d):
            a, b = (cs64_d, ss64_d) if d == 64 else (cs32_d, ss32_d)
            dma("sp", TAB[0], a, [], [T_tab])
            dma("sp", TAB[1], b, [], [T_tab])

        st_rr = [0]
        op_rr = [0]
        pt_rr = [0]
        ST_BANKS = [0, 1, 2]
        OP_BANKS = [3, 4, 5, 6]
        TR_BANK = 7

        pend = []
        SKEW = 2

        def pipe_push(fn, skew=SKEW):
            pend.append([fn, []])
            while len(pend) > skew:
                f, cbs = pend.pop(0)
                f()
                for c in cbs:
                    c()

        def pipe_cb(fn):
            if pend:
                pend[-1][1].append(fn)
            else:
                fn()

        def pipe_flush():
            while pend:
                f, cbs = pend.pop(0)
                f()
                for c in cbs:
                    c()

        st_banks = {"l": [0, 1, 2]}
        pad_rr = [0]

        def pk_zero():
            for i in range(4):
                memset("pool", PK[i], 0.0, [], [T_pk[i]])


        def attn_group(m, g, kbs, vheads):
            obs = []
            for _ in vheads:
                obs.append(OP_BANKS[op_rr[0] % 4])
                op_rr[0] += 1
            SKEW_ = len(vheads) * (2 if len(vheads) <= 2 else 1)
            first = True
            for kb in kbs:
                if m == "A":
                    qbs = [qb for qb in range(4 * g, 4 * g + 4) if kb <= qb <= kb + 4]
                else:
                    qbs = [qb for qb in range(4 * g, 4 * g + 4) if qb >= kb]
                if not qbs:
                    continue
                n = len(qbs) * 128
                q0 = qbs[0] * 128
                for vi, vhd in enumerate(vheads):
                    stl = st_banks["l"]
                    sb_i = stl[st_rr[0] % len(stl)]
                    st_rr[0] += 1
                    pt_i = pt_rr[0] % 6
                    pt_rr[0] += 1
                    base, rows, tile = vhd["base"], vhd["rows"], vhd["tile"]
                    if vhd.get("pad"):
                        pos = base // 32
                        ceng = vhd["pad"][pad_rr[0] % len(vhd["pad"])]
                        pad_rr[0] += 1
                        cp(ceng, PK[pos][base:base + rows, :], KT[base:base + rows, tile, kb * 128:(kb + 1) * 128],
                           [vhd["kt"][kb // 4]], [T_pk[pos]])
                        mm(psb[sb_i][:, 0:n], PK[pos], QT[:, tile, q0:q0 + n], True, True,
                           [vhd["qt"][g], T_pk[pos]], [T_ps[sb_i]])
                    else:
                        lhsT = KT[base:base + rows, tile, kb * 128:(kb + 1) * 128]
                        rhs = QT[base:base + rows, tile, q0:q0 + n]
                        kw = {}
                        if base == 96:
                            kw["tile_position"] = (96, 0)
                        mm(psb[sb_i][:, 0:n], lhsT, rhs, True, True, [vhd["qt"][g], vhd["kt"][kb // 4]], [T_ps[sb_i]], **kw)
                    vhd["exp"](kb, qbs, sb_i, pt_i)
                    vhd["post"](kb, qbs, pt_i)

                    def pv(kb=kb, qbs=qbs, pt_i=pt_i, first=first, n=n, ob=obs[vi], vh=vhd["vh"]):
                        c0 = (qbs[0] - 4 * g) * 128
                        mm(psb[ob][:, c0:c0 + n], V[:, kb, vh, :], PT[pt_i][:, 0:n],
                           first, False, [T_PT[pt_i], T_V[kb]], [T_ps[ob]], skip_group_check=True)
                    pipe_push(pv, SKEW_)
                    bg_tick()
                first = False
            return obs

        NB = TR_BANK
        bg = {"chunks": [], "per": 0}

        def bg_tick():
            for _ in range(bg["per"]):
                if bg["chunks"]:
                    bg["chunks"].pop(0)()

        def bg_drain():
            while bg["chunks"]:
                bg["chunks"].pop(0)()
            bg["per"] = 0

        def norm_fm(ob, mi, h, g):
            k = rope_rr[0] % 4
            rope_rr[0] += 1
            act(tmp[k][0:64, :], psb[ob][64:128, :], AF.Ln, [T_ps[ob]], [T_tmp[k]])
            act(tmp[k][0:64, :], tmp[k][0:64, :], AF.Exp, [T_tmp[k]], [T_tmp[k]], scale=-1.0)
            r0 = (h % 2) * 64
            tt("dve", mixT[r0:r0 + 64, 2 * mi + h // 2, g * 512:(g + 1) * 512], psb[ob][0:64, :], tmp[k][0:64, :], ALU.mult,
               [T_ps[ob], T_tmp[k]], [T_mix[mi][g]])

        T_tmpX = [[Tok() for _ in range(4)] for _ in range(2)]

        def d_fork():
            for k in range(4):
                memset("pool", tmp[k][0:1, 0:1], 0.0, [T_tmp[k]], [T_tmp[k], T_tmpX[0][k], T_tmpX[1][k]])

        def d_join():
            for k in range(4):
                memset("pool", tmp[k][0:1, 0:1], 0.0, [T_tmpX[0][k], T_tmpX[1][k]], [T_tmp[k]])

        def d_combine_fm(obs, mi, h, g):
            st_ = h % 2
            p0 = st_ * 64
            tk = T_tmpX[st_]
            t0, t1, t2, t3 = [t[p0:p0 + 64, :] for t in tmp]
            o1 = psb[obs[0]]
            o2 = psb[obs[1]]
            act(t0, o1[64:128, :], AF.Ln, [T_ps[obs[0]]], [tk[0]])
            act(t0, t0, AF.Exp, [tk[0]], [tk[0]], scale=-1.0)
            act(t1, o2[64:128, :], AF.Ln, [T_ps[obs[1]]], [tk[1]])
            act(t1, t1, AF.Exp, [tk[1]], [tk[1]], scale=-1.0)
            tt("dve", t2, o1[0:64, :], t0, ALU.mult, [T_ps[obs[0]], tk[0]], [tk[2]])
            tt("dve", t3, o2[0:64, :], t1, ALU.mult, [T_ps[obs[1]], tk[1]], [tk[3]])
            stt(t2, t3, nlam_t[p0:p0 + 64, :], t2, ALU.mult, ALU.add, [tk[3], tk[2], T_cst], [tk[2]])
            tt("pool", t3, t2, t2, ALU.mult, [tk[2]], [tk[3]])
            mm(psb[NB][0:64, :], ones_f[p0:p0 + 64, 0:64], t3, True, True, [tk[3], T_misc], [T_ps[NB]])
            act(t0, psb[NB][0:64, :], AF.Ln, [T_ps[NB], T_cst], [tk[0]], scale=1.0 / 64, bias=eps_t[p0:p0 + 64, :])
            act(t1, t0, AF.Exp, [tk[0]], [tk[1]], scale=-0.5)
            r0 = (h % 2) * 64
            stt(mixT[r0:r0 + 64, 2 * mi + h // 2, g * 512:(g + 1) * 512], t2, dngc2[p0:p0 + 64, :], t1, ALU.mult, ALU.mult,
                [tk[2], tk[1], T_cst], [T_mix[mi][g]])

        otm_rr = [0]

        for l in range(n_layers):
            lam_init = 0.8 - 0.6 * math.exp(-0.3 * l)
            if l == 0:
                for tb in range(NT):
                    dma("sp", X[:, tb, :], x_d[tb * 128:(tb + 1) * 128, :], [], [T_X[tb]])
            dma("sp", relc_t, bcast_rows(relc_d[l:l + 1, :]), [], [T_cst])
            dma("sp", fb_t, bcast_rows(fb_d[l:l + 1, :]), [], [T_cst])
            dma("sp", lamv, bcast_rows(lam_d[l:l + 1, :]), [], [T_misc])
            dma("sp", dngT, bcast_rows(dng_d[l:l + 1, :]), [], [T_cst])
            dma("sp", relbT, relb_d[l].rearrange("k (h d q) -> k h d q", h=4, d=2), [], [T_relb])
            tt("dve", lamw, lamv[:, 0:32], lamv[:, 32:64], ALU.mult, [T_misc], [T_cst])
            S.add("dve", lambda e: e.tensor_reduce(out=cst[:, 48:49], in_=lamw, axis=AX.X, op=ALU.add), [T_cst], [T_cst])
            tt("dve", lamw, lamv[:, 64:96], lamv[:, 96:128], ALU.mult, [T_misc, T_cst], [T_cst])
            S.add("dve", lambda e: e.tensor_reduce(out=cst[:, 49:50], in_=lamw, axis=AX.X, op=ALU.add), [T_cst], [T_cst])
            act(cst[:, 50:52], cst[:, 48:50], AF.Exp, [T_cst], [T_cst])
            tt("dve", lam_t, cst[:, 50:51], cst[:, 51:52], ALU.subtract, [T_cst], [T_cst])
            ts("dve", lam_t, lam_t, lam_init, None, ALU.add, None, [T_cst], [T_cst])
            ts("dve", nlam_t, lam_t, -1.0, None, ALU.mult, None, [T_cst], [T_cst])
            ts("dve", dngT, dngT, 1.0 - lam_init, None, ALU.mult, None, [T_cst], [T_cst])
            dma("sp", dngc2[0:64, :], dng_d[l].rearrange("(d o) -> d o", o=1), [], [T_cst])
            dma("sp", dngc2[64:128, :], dng_d[l].rearrange("(d o) -> d o", o=1), [], [T_cst])
            ts("dve", dngc2, dngc2, 1.0 - lam_init, None, ALU.mult, None, [T_cst], [T_cst])

            norm_to_hT(l, g1_d[l:l + 1, :])
            if l > 0:
                for tb in range(NT):
                    dma("sp", xres_d[tb * 128:(tb + 1) * 128, :], X[:, tb, :], [T_X[tb]], [T_xres])
            if debug and l == 0:
                dma("sp", dbg_h, hT.rearrange("p a b -> p (a b)"), T_hT, [T_dbg])
            S.barrier()
            memset("pool", V[:, :, :, 64:128], 1.0, [], T_V)

            for mi_, m in enumerate(order):
                mi = "ABCD".index(m)
                if m in "AC":
                    s0 = w_get()
                    proj_feature(s0, 4, plain_evac(
                        lambda ci, tg: (QT if ci < 2 else KT)[:, ci % 2, tg * 512:(tg + 1) * 512],
                        lambda ci, tg: (T_QT if ci < 2 else T_KT)[ci % 2][tg]))
                    w_release()
                    s1 = w_get()
                    if m == "A":
                        proj_token(s1, 256, v_evac())
                    else:
                        def fg_extra(tb, pi):
                            cp("dve", fgT[:, tb, :], psb[pi][:, 256:260], [T_ps[pi]], [T_misc])
                        proj_token(s1, 260, v_evac(fg_extra))
                    w_release()
                elif m == "D":
                    load_tables(32)
                    for (dst, dtk) in ((QT, T_QT), (KT, T_KT)):
                        s0 = w_get()
                        proj_feature_pairs(s0, 2, rope_evac(
                            lambda pj, tg, dst=dst: dst[:, pj, tg * 512:(tg + 1) * 512],
                            lambda pj, tg, dtk=dtk: dtk[pj][tg]))
                        w_release()
                    s1 = w_get()
                    proj_token(s1, 256, v_evac())
                    w_release()
                elif m == "B":
                    load_tables(64)
                    for (dst, dtk) in ((QT, T_QT), (KT, T_KT)):
                        s0 = w_get()
                        proj_feature_pairs(s0, 2, rope_evac(
                            lambda pj, tg, dst=dst: dst[:, pj, tg * 512:(tg + 1) * 512],
                            lambda pj, tg, dtk=dtk: dtk[pj][tg]))
                        w_release()
                    load_tables(32)
                    s0 = w_get()
                    proj_feature_pairs(s0, 2, rope_evac(
                        lambda pj, tg: qiT[:, pj, tg * 512:(tg + 1) * 512],
                        lambda pj, tg: T_qi[pj][tg]))
                    w_release()
                    s0 = w_get()
                    proj_feature_pairs(s0, 1, rope_evac(
                        lambda pj, tg: kiT[:, tg * 512:(tg + 1) * 512],
                        lambda pj, tg: T_ki[tg]))
                    w_release()
                    s1 = w_get()

                    def wi_extra(tb, pi):
                        cp("dve", wiT[:, tb, :], psb[pi][:, 256:264], [T_ps[pi]], [T_misc])
                    proj_token(s1, 264, v_evac(wi_extra))
                    w_release()

                if m == "A":
                    def a_exp(h):
                        def f(kb, qbs, sb_i, pt_i):
                            far = [qb for qb in qbs if qb - kb >= 2]
                            for j, qb in enumerate(qbs):
                                dl = qb - kb
                                if dl <= 1:
                                    k = rope_rr[0] % 4
                                    rope_rr[0] += 1
                                    stt(tmp[k][:, 0:128], psb[sb_i][:, j * 128:(j + 1) * 128], 0.125, relbT[:, h, dl, :],
                                        ALU.mult, ALU.add, [T_ps[sb_i], T_relb], [T_tmp[k]])
                                    act(PT[pt_i][:, j * 128:(j + 1) * 128], tmp[k][:, 0:128], AF.Exp, [T_tmp[k]], [T_PT[pt_i]])
                            if far:
                                j0 = qbs.index(far[0])
                                act(PT[pt_i][:, j0 * 128:(j0 + len(far)) * 128], psb[sb_i][:, j0 * 128:(j0 + len(far)) * 128], AF.Exp,
                                    [T_ps[sb_i], T_cst], [T_PT[pt_i]], scale=0.125, bias=relc_t[:, h:h + 1])
                        return f

                    def a_post(kb, qbs, pt_i):
                        for j, qb in enumerate(qbs):
                            if qb == kb:
                                memset("pool", PT[pt_i][64:128, j * 128:j * 128 + 64], 0.0, [T_PT[pt_i]], [T_PT[pt_i]])
                            if qb == kb + 4:
                                memset("pool", PT[pt_i][0:64, j * 128 + 64:j * 128 + 128], 0.0, [T_PT[pt_i]], [T_PT[pt_i]])

                    st_banks["l"] = [0, 1, 2, 7]
                    pk_zero()
                    for g in range(4):
                        kbs = list(range(max(0, 4 * g - 4), 4 * g + 4))
                        for hp in range(2):
                            vhs = [dict(tile=hp, base=(h % 2) * 64, rows=64, vh=h, exp=a_exp(h), post=a_post, qt=T_QT[hp], kt=T_KT[hp],
                                        pad=("dve",))
                                   for h in (2 * hp, 2 * hp + 1)]
                            obs = attn_group("A", g, kbs, vhs)
                            for ob, h in zip(obs, (2 * hp, 2 * hp + 1)):
                                pipe_cb(lambda ob=ob, h=h, g=g: norm_fm(ob, mi, h, g))
                    pipe_flush()
                    st_banks["l"] = [0, 1, 2]

                elif m == "C":
                    fg2 = fgT.rearrange("p a b -> p (a b)")
                    tt("dve", fgT, fgT, fb_t.unsqueeze(1).to_broadcast([128, NT, 4]), ALU.add, [T_misc, T_cst], [T_misc])
                    act(fg2, fg2, AF.Exp, [T_misc], [T_misc], scale=-1.0)
                    act(fg2, fg2, AF.Ln, [T_misc], [T_misc], bias=1.0)
                    ts("dve", fg2, fg2, -1.0, None, ALU.mult, None, [T_misc], [T_misc])
                    memset("dve", gexT[:, 0, :], 0.0, [], [T_misc])
                    for tb in range(1, NT):
                        tt("dve", gexT[:, tb, :], gexT[:, tb - 1, :], fgT[:, tb - 1, :], ALU.add, [T_misc], [T_misc])
                    pi = next_ps()
                    pj = next_ps()
                    gex2 = gexT.rearrange("p a b -> p (a b)")
                    mm(psb[pi][:, 0:64], tri, fg2, True, False, [T_misc], [T_ps[pi]])
                    mm(psb[pi][:, 0:64], ones_f, gex2, False, True, [T_misc], [T_ps[pi]])
                    mm(psb[pj][:, 0:64], mhalf, fg2, True, False, [T_misc], [T_ps[pj]])
                    mm(psb[pj][:, 0:64], ones_f, gex2, False, True, [T_misc], [T_ps[pj]])
                    cp("dve", cumT.rearrange("p a b -> p (a b)"), psb[pi][:, 0:64], [T_ps[pi]], [T_misc])
                    cp("dve", cbT.rearrange("p a b -> p (a b)"), psb[pj][:, 0:64], [T_ps[pj]], [T_misc])
                    pair_idx = {}
                    pidx = 0
                    for qb in range(NT):
                        for h in range(4):
                            ts("dve", biasC[:, h, pidx:pidx + qb + 1], cumT[:, 0:qb + 1, h], -1.0, cbT[:, qb, h:h + 1],
                               ALU.mult, ALU.add, [T_misc], [T_misc])
                        for kb in range(qb + 1):
                            pair_idx[(qb, kb)] = pidx + kb
                        pidx += qb + 1

                    def c_exp(h):
                        def f(kb, qbs, sb_i, pt_i):
                            for j, qb in enumerate(qbs):
                                act(PT[pt_i][:, j * 128:(j + 1) * 128], psb[sb_i][:, j * 128:(j + 1) * 128], AF.Exp,
                                    [T_ps[sb_i], T_misc], [T_PT[pt_i]], scale=0.125,
                                    bias=biasC[:, h, pair_idx[(qb, kb)]:pair_idx[(qb, kb)] + 1])
                        return f

                    def c_post(kb, qbs, pt_i):
                        for j, qb in enumerate(qbs):
                            if qb == kb:
                                ap = PT[pt_i][:, j * 128:(j + 1) * 128]
                                S.add("pool", lambda e, ap=ap: e.affine_select(out=ap, in_=ap, pattern=[[1, 128]], compare_op=ALU.is_ge,
                                                                               fill=0.0, base=0, channel_multiplier=-1),
                                      [T_PT[pt_i]], [T_PT[pt_i]])

                    st_banks["l"] = [0, 1, 2, 7]
                    pk_zero()
                    for g in range(4):
                        for hp in range(2):
                            vhs = [dict(tile=hp, base=(h % 2) * 64, rows=64, vh=h, exp=c_exp(h), post=c_post, qt=T_QT[hp], kt=T_KT[hp],
                                        pad=("dve",))
                                   for h in (2 * hp, 2 * hp + 1)]
                            obs = attn_group("C", g, list(range(4 * g + 4)), vhs)
                            for ob, h in zip(obs, (2 * hp, 2 * hp + 1)):
                                pipe_cb(lambda ob=ob, h=h, g=g: norm_fm(ob, mi, h, g))
                    pipe_flush()
                    st_banks["l"] = [0, 1, 2]

                elif m in "DB":
                    sc = 32 ** -0.5 if m == "D" else 0.125

                    def d_exp(kb, qbs, sb_i, pt_i):
                        n = len(qbs) * 128
                        act(PT[pt_i][:, 0:n], psb[sb_i][:, 0:n], AF.Exp, [T_ps[sb_i]], [T_PT[pt_i]], scale=sc)

                    def d_post(kb, qbs, pt_i):
                        for j, qb in enumerate(qbs):
                            if qb == kb:
                                memset("pool", PT[pt_i][64:128, j * 128:j * 128 + 64], 0.0, [T_PT[pt_i]], [T_PT[pt_i]])

                    if m == "D":
                        d_fork()
                        pk_zero()
                        for g in range(4):
                            for h in range(4):
                                hp = h // 2
                                vhs = [dict(tile=hp, base=((h % 2) * 2 + r) * 32, rows=32, vh=h, exp=d_exp, post=d_post,
                                            qt=T_QT[hp], kt=T_KT[hp], pad=("dve",)) for r in range(2)]
                                obs = attn_group("D", g, list(range(4 * g + 4)), vhs)
                                pipe_cb(lambda obs=obs, h=h, g=g: d_combine_fm(obs, mi, h, g))
                        pipe_flush()
                        d_join()
                    else:
                        S.barrier()
                        KSEL = 256.0
                        selTs = [carve(0, [128, 12, 512], BF16), carve(12288, [128, 16, 512], BF16)]
                        junkb2 = carve(28672, [128, SEQ], BF16)
                        uleft = carve(U + 48768, [128, 3456], F32)
                        scb = [uleft[:, 1792:3456], uleft[:, 0:1792], TAB[1], TAB[0]]
                        selb2 = carve(W + 32768, [128, SEQ], BF16)
                        T_sc = [Tok() for _ in range(4)]
                        T_bq = [Tok() for _ in range(4)]
                        T_sT = [[Tok() for _ in range(4)] for _ in range(2)]
                        T_selb = Tok()

                        ajunk = G.bitcast(BF16)
                        ACT_CHAINS = (0, 1)

                        def b_select_chunks(g):
                            chunks = []
                            sT = selTs[g % 2]
                            tks = T_sT[g % 2]
                            active = []
                            for jq in range(4):
                                qb = 4 * g + jq
                                if qb < 2:
                                    chunks.append(lambda jq=jq, qb=qb: memset("pool", sT[:, 0:qb + 1, jq * 128:(jq + 1) * 128], 1.0, [], [tks[jq]]))
                                else:
                                    active.append((jq, qb))

                            def score_piece(jq, qb, gi, c0, n):
                                base = (gi % 4) * 32
                                kw = {"tile_position": (96, 0)} if base == 96 else {}
                                sb_i = ST_BANKS[st_rr[0] % 3]
                                st_rr[0] += 1
                                k = rope_rr[0] % 4
                                rope_rr[0] += 1
                                mm(psb[sb_i][:, 0:n], qiT[base:base + 32, gi // 4, qb * 128:(qb + 1) * 128],
                                   kiT[base:base + 32, c0:c0 + n], True, True,
                                   [T_qi[gi // 4][g]] + T_ki, [T_ps[sb_i]], **kw)
                                act(tmp[k][:, 0:n], psb[sb_i][:, 0:n], AF.Relu, [T_ps[sb_i]], [T_tmp[k]])
                                if gi == 0:
                                    ts("dve", scb[jq][:, c0:c0 + n], tmp[k][:, 0:n], wiT[:, qb, 0:1], None, ALU.mult, None,
                                       [T_tmp[k], T_misc], [T_sc[jq]])
                                else:
                                    stt(scb[jq][:, c0:c0 + n], tmp[k][:, 0:n], wiT[:, qb, gi:gi + 1], scb[jq][:, c0:c0 + n],
                                        ALU.mult, ALU.add, [T_tmp[k], T_misc, T_sc[jq]], [T_sc[jq]])

                            for jq, qb in active:
                                nk = (qb + 1) * 128
                                for gi in range(8):
                                    for c0 in range(0, nk, 512):
                                        n = min(512, nk - c0)
                                        chunks.append(lambda jq=jq, qb=qb, gi=gi, c0=c0, n=n: score_piece(jq, qb, gi, c0, n))
                                chunks.append(lambda jq=jq, qb=qb: memset("dve", scb[jq][0:64, qb * 128 + 64:qb * 128 + 128], -1.0e30,
                                                                          [T_sc[jq]], [T_sc[jq]]))

                            def init_piece():
                                for jq, qb in active:
                                    nk = (qb + 1) * 128
                                    memset("pool", bis[:, 4 * jq:4 * jq + 1], 0.0, [T_bq[jq]], [T_bq[jq]])
                                    if jq in ACT_CHAINS:
                                        memset("pool", bis[:, 4 * jq + 3:4 * jq + 4], float(nk) - 2.0 * KSEL + 0.5, [T_bq[jq]], [T_bq[jq]])
                            chunks.append(init_piece)

                            def round_piece(step):
                                for jq, qb in active:
                                    nk = (qb + 1) * 128
                                    c = 4 * jq
                                    if jq in ACT_CHAINS:
                                        act(ajunk[:, 0:nk], scb[jq][:, 0:nk], AF.Sign, [T_sc[jq], T_bq[jq]], [T_bq[jq]],
                                            bias=bis[:, c:c + 1], accum_out=bis[:, c + 1:c + 2])
                                    else:
                                        ts("dve", junkb2[:, 0:nk], scb[jq][:, 0:nk], bis[:, c:c + 1], None, ALU.is_ge, ALU.add,
                                           [T_sc[jq], T_bq[jq]], [T_bq[jq]], accum_out=bis[:, c + 1:c + 2])
                                for jq, qb in active:
                                    c = 4 * jq
                                    if jq in ACT_CHAINS:
                                        act(bis[:, c + 2:c + 3], bis[:, c + 1:c + 2], AF.Sign, [T_bq[jq]], [T_bq[jq]], bias=bis[:, c + 3:c + 4])
                                    else:
                                        ts("dve", bis[:, c + 2:c + 3], bis[:, c + 1:c + 2], KSEL, 2.0 * step,
                                           ALU.is_ge, ALU.mult, [T_bq[jq]], [T_bq[jq]])
                                for jq, qb in active:
                                    c = 4 * jq
                                    if jq in ACT_CHAINS:
                                        act(bis[:, c:c + 1], bis[:, c + 2:c + 3], AF.Identity, [T_bq[jq]], [T_bq[jq]],
                                            scale=-step, bias=bis[:, c:c + 1])
                                    else:
                                        stt(bis[:, c:c + 1], bis[:, c + 2:c + 3], -step, bis[:, c:c + 1],
                                            ALU.add, ALU.add, [T_bq[jq]], [T_bq[jq]])

                            step = 64.0
                            for it in range(nbis):
                                chunks.append(lambda step=step: round_piece(step))
                                step *= 0.5
                            fstep = step

                            def final_piece(jq, qb):
                                nk = (qb + 1) * 128
                                mid = bis[:, 4 * jq:4 * jq + 1]
                                if jq in ACT_CHAINS:
                                    ts("dve", mid, mid, -1.0, -2.0 * fstep, ALU.mult, ALU.add, [T_bq[jq]], [T_bq[jq]])
                                else:
                                    ts("dve", mid, mid, -2.0 * fstep, None, ALU.add, None, [T_bq[jq]], [T_bq[jq]])
                                ts("dve", selb2[:, 0:nk], scb[jq][:, 0:nk], mid, None, ALU.is_ge, None, [T_sc[jq], T_bq[jq]], [T_selb])
                                for k0 in range(0, qb + 1, 4):
                                    kn = min(4, qb + 1 - k0)
                                    pT = psb[TR_BANK][:, :].bitcast(BF16)
                                    for kk in range(kn):
                                        tr(pT[:, kk * 128:(kk + 1) * 128], selb2[:, (k0 + kk) * 128:(k0 + kk + 1) * 128],
                                           [T_selb, T_cst], [T_ps[TR_BANK]])
                                    cp("act", sT[:, k0:k0 + kn, jq * 128:(jq + 1) * 128],
                                       pT[:, 0:kn * 128].rearrange("p (a b) -> p a b", a=kn), [T_ps[TR_BANK]], [tks[jq]])
                            for jq, qb in active:
                                chunks.append(lambda jq=jq, qb=qb: final_piece(jq, qb))
                            return chunks

                        def b_attend(g):
                            sT = selTs[g % 2]
                            tks = T_sT[g % 2]

                            def b_post(kb, qbs, pt_i):
                                n = len(qbs) * 128
                                j0 = qbs[0] - 4 * g
                                for j, qb in enumerate(qbs):
                                    if qb == kb:
                                        memset("pool", PT[pt_i][64:128, j * 128:j * 128 + 64], 0.0, [T_PT[pt_i]], [T_PT[pt_i]])
                                tt("pool", PT[pt_i][:, 0:n], PT[pt_i][:, 0:n], sT[:, kb, j0 * 128:j0 * 128 + n], ALU.mult,
                                   [T_PT[pt_i]] + tks, [T_PT[pt_i]])

                            for hp in range(2):
                                vhs = [dict(tile=hp, base=(h % 2) * 64, rows=64, vh=h, exp=d_exp, post=b_post, qt=T_QT[hp], kt=T_KT[hp])
                                       for h in (2 * hp, 2 * hp + 1)]
                                obs = attn_group("B", g, list(range(4 * g + 4)), vhs)
                                for ob, h in zip(obs, (2 * hp, 2 * hp + 1)):
                                    pipe_cb(lambda ob=ob, h=h, g=g: norm_fm(ob, mi, h, g))
                            pipe_flush()

                        for c in b_select_chunks(0):
                            c()
                        for g in range(4):
                            if g + 1 < 4:
                                bg["chunks"] = b_select_chunks(g + 1)
                                nsteps = 4 * (4 * g + 4)
                                bg["per"] = -(-len(bg["chunks"]) // nsteps)
                            b_attend(g)
                            bg_drain()

            if debug and l == 0:
                S.barrier()
                dma("sp", dbg_m, mixT.rearrange("p a b -> p (a b)"), [t for tt_ in T_mix for t in tt_], [T_dbg])

            S.barrier()
            xsrc = x_d if l == 0 else xres_d
            for tb in range(NT):
                dma("sp", X[:, tb, :], xsrc[tb * 128:(tb + 1) * 128, :], [T_xres], [T_X[tb]])
            s0 = w_get()
            s1 = w_get()
            for tb in range(NT):
                for hf, sl in enumerate((s0, s1)):
                    pi = next_ps()
                    for kc in range(8):
                        mm(psb[pi][:, :], mixT[:, kc, tb * 128:(tb + 1) * 128], wunit[sl][:, kc, :], kc == 0, kc == 7,
                           [T_wu[sl], T_mix[kc // 2][tb // 4]], [T_ps[pi]])
                    tt("dve", X[:, tb, hf * 512:(hf + 1) * 512], X[:, tb, hf * 512:(hf + 1) * 512], psb[pi][:, :], ALU.add,
                       [T_ps[pi], T_X[tb]], [T_X[tb]])
            w_release()
            w_release()
            if debug and l == 0 and not do_ffn:
                for tb in range(NT):
                    dma("sp", dbg_x[tb * 128:(tb + 1) * 128, :], X[:, tb, :], [T_X[tb]], [T_dbg])

            if do_ffn:
                norm_to_hT(l, g2_d[l:l + 1, :])
                actT = mixT
                T_act = [Tok() for _ in range(NT)]
                for (f0, nf) in ((0, 1024), (1024, 1024), (2048, 768)):
                    nfc = nf // 128
                    fc = 0
                    for c0 in range(f0, f0 + nf, 512):
                        nn = min(512, f0 + nf - c0)
                        sg = w_get()
                        su = w_get()
                        for ci in range(nn // 128):
                            for tg in range(4):
                                pg = next_ps()
                                pu = next_ps()
                                for kc in range(8):
                                    mm(psb[pg][:, :], wunit[sg][:, kc, ci * 128:(ci + 1) * 128], hT[:, kc, tg * 512:(tg + 1) * 512],
                                       kc == 0, kc == 7, [T_wu[sg]] + T_hT[tg * 4:tg * 4 + 4], [T_ps[pg]])
                                for kc in range(8):
                                    mm(psb[pu][:, :], wunit[su][:, kc, ci * 128:(ci + 1) * 128], hT[:, kc, tg * 512:(tg + 1) * 512],
                                       kc == 0, kc == 7, [T_wu[su]] + T_hT[tg * 4:tg * 4 + 4], [T_ps[pu]])
                                k = rope_rr[0] % 4
                                rope_rr[0] += 1
                                act(tmp[k], psb[pg][:, :], AF.Silu, [T_ps[pg]], [T_tmp[k]])
                                tt("dve", actT[:, fc, tg * 512:(tg + 1) * 512], psb[pu][:, :], tmp[k], ALU.mult,
                                   [T_ps[pu], T_tmp[k]], T_act[tg * 4:tg * 4 + 4])
                            fc += 1
                        w_release()
                        w_release()
                    for hf in range(2):
                        sd = w_get()
                        for tb in range(NT):
                            pi = next_ps()
                            for k2 in range(nfc):
                                mm(psb[pi][:, :], actT[:, k2, tb * 128:(tb + 1) * 128], wunit[sd][:, k2, :], k2 == 0, k2 == nfc - 1,
                                   [T_wu[sd], T_act[tb]], [T_ps[pi]])
                            tt("dve", X[:, tb, hf * 512:(hf + 1) * 512], X[:, tb, hf * 512:(hf + 1) * 512], psb[pi][:, :], ALU.add,
                               [T_ps[pi], T_X[tb]], [T_X[tb]])
                        w_release()
                if debug and l == 0:
                    for tb in range(NT):
                        dma("sp", dbg_x[tb * 128:(tb + 1) * 128, :], X[:, tb, :], [T_X[tb]], [T_dbg])

        if do_final:
            S.barrier()
            dma("sp", G, bcast_rows(gf_d[0:1, :]), [], [T_G])
            T_ob = [Tok(), Tok()]
            for tb in range(NT):
                b = tb % 2
                ssq = small[:, tb:tb + 1]
                std = small[:, 16 + tb:17 + tb]
                rstd = small[:, 32 + tb:33 + tb]
                act(junk, X[:, tb, :], AF.Square, [T_X[tb]], [T_junk, T_ss[tb]], accum_out=ssq)
                act(std, ssq, AF.Sqrt, [T_ss[tb], T_cst], [T_ss[tb]], scale=1.0 / DM, bias=eps_t)
                S.add("dve", lambda e, o=rstd, i=std: e.reciprocal(out=o, in_=i), [T_ss[tb]], [T_ss[tb]])
                stt(obuf[b], X[:, tb, :], rstd, G, ALU.mult, ALU.mult, [T_X[tb], T_ss[tb], T_G], [T_ob[b]])
                dma("sp", out_d[tb * 128:(tb + 1) * 128, :], obuf[b], [T_ob[b]], [T_out])
        else:
            for tb in range(NT):
                dma("sp", out_d[tb * 128:(tb + 1) * 128, :], X[:, tb, :], [T_X[tb]], [T_out])

        stats = S.emit(sems, dsems)
    return nc, stats


def _prep_shared(ln1_g, w_in, rel_bias, forget_b, lam_q1, lam_k1, lam_q2, lam_k2, diff_norm_g, w_o, ln2_g,
                 w_gate, w_up, w_down, final_g):
    f = lambda a: np.ascontiguousarray(np.asarray(a, dtype=np.float32))
    sh = {}
    sh["win"] = f(np.asarray(w_in)[:, :, WIN_COLS])
    sh["wo"] = f(w_o)
    sh["wg"] = f(w_gate)
    sh["wu"] = f(w_up)
    sh["wd"] = f(w_down)
    sh["g1"] = f(ln1_g)
    sh["g2"] = f(ln2_g)
    sh["gf"] = f(np.asarray(final_g).reshape(1, DM))
    rb = np.asarray(rel_bias, dtype=np.float32)
    k = np.arange(128)[:, None]
    q = np.arange(128)[None, :]
    tiles = []
    for dl in range(2):
        idx = np.clip(q - k + 128 * dl, -128, 128) + 128
        tiles.append(rb[:, :, idx])
    t = np.stack(tiles, axis=2)
    sh["relb"] = f(np.transpose(t, (0, 3, 1, 2, 4)).reshape(NL, 128, 4 * 2 * 128))
    sh["relc"] = f(rb[:, :, 256])
    sh["fb"] = f(forget_b)
    sh["lamv"] = f(np.concatenate([np.asarray(lam_q1), np.asarray(lam_k1), np.asarray(lam_q2), np.asarray(lam_k2)], axis=1))
    sh["dng"] = f(diff_norm_g)
    sh["cs64"], sh["ss64"] = _rope_tables(64)
    sh["cs32"], sh["ss32"] = _rope_tables(32)
    return sh


_CACHE = {}


def kernel(x, ln1_g, w_in, rel_bias, forget_b, lam_q1, lam_k1, lam_q2, lam_k2, diff_norm_g, w_o, ln2_g,
           w_gate, w_up, w_down, final_g):
    x = np.asarray(x, dtype=np.float32)
    sh = _prep_shared(ln1_g, w_in, rel_bias, forget_b, lam_q1, lam_k1, lam_q2, lam_k2, diff_norm_g, w_o, ln2_g,
                      w_gate, w_up, w_down, final_g)
    if "nc" not in _CACHE:
        _CACHE["nc"] = build_program()[0]
    nc = _CACHE["nc"]
    in_maps = []
    for c in range(NCORES):
        mp = dict(sh)
        mp["x"] = np.ascontiguousarray(x[c])
        in_maps.append(mp)
    res = run_bass_kernel_spmd(nc, in_maps, core_ids=list(range(NCORES)))
    out = np.stack([np.asarray(res.results[c]["out"], dtype=np.float32).reshape(SEQ, DM) for c in range(NCORES)], axis=0)
    return out
```

```python
import math
from contextlib import ExitStack

import numpy as np
import concourse.bass as bass
import concourse.mybir as mybir
from concourse.bass_utils import run_bass_kernel_spmd

F32 = mybir.dt.float32
BF16 = mybir.dt.bfloat16
AF = mybir.ActivationFunctionType
ALU = mybir.AluOpType
AX = mybir.AxisListType

ENGS = ("pe", "act", "dve", "pool", "sp")

SEQ = 2048
DM = 1024
NT = 16
DFF = 2816
NL = 2
EPS = 1e-6
NCORES = 8


class Tok:
    __slots__ = ("name", "w", "r")

    def __init__(self, name=""):
        self.name = name
        self.w = None
        self.r = []


class Op:
    __slots__ = ("eng", "fn", "deps", "signal", "seq", "is_dma", "dsem", "dval")

    def __init__(self, eng, fn, is_dma=False):
        self.eng = eng
        self.fn = fn
        self.deps = []
        self.signal = False
        self.seq = None
        self.is_dma = is_dma
        self.dsem = None
        self.dval = None


class Sched:
    def __init__(self, nc, n_dma_sems=8):
        self.nc = nc
        self.q = {e: [] for e in ENGS}
        self.n_dma_sems = n_dma_sems

    def add(self, eng, fn, reads=(), writes=(), is_dma=False):
        op = Op(eng, fn, is_dma)
        deps = []
        for t in reads:
            if t.w is not None:
                deps.append((t.w, "raw"))
        for t in writes:
            if t.w is not None:
                deps.append((t.w, "waw"))
            for r in t.r:
                deps.append((r, "war"))
        seen = set()
        for d, kind in deps:
            if d is op or id(d) in seen:
                continue
            if d.eng == eng and not d.is_dma and not is_dma:
                if eng in ("pe", "sp"):
                    continue
                if kind != "raw":
                    continue
            seen.add(id(d))
            op.deps.append(d)
            d.signal = True
        for t in reads:
            t.r.append(op)
        for t in writes:
            t.w = op
            t.r = []
        if is_dma:
            op.signal = True
        self.q[eng].append(op)
        return op

    def barrier(self):
        lasts = []
        for e in ENGS:
            ql = self.q[e]
            for o in reversed(ql):
                if not o.is_dma and o.fn is not None:
                    lasts.append(o)
                    break
            cnt = 0
            for o in reversed(ql):
                if o.is_dma:
                    lasts.append(o)
                    cnt += 1
                    if cnt >= self.n_dma_sems:
                        break
        for e in ENGS:
            op = Op(e, None)
            for d in lasts:
                if d.eng == e and not d.is_dma:
                    continue
                op.deps.append(d)
                d.signal = True
            self.q[e].append(op)

    def emit(self, sems, dma_sems):
        nc = self.nc
        for e in ENGS:
            c = 0
            k = 0
            for o in self.q[e]:
                if o.is_dma:
                    o.dsem = dma_sems[e][k % self.n_dma_sems]
                    o.dval = 16 * (k // self.n_dma_sems + 1)
                    k += 1
                elif o.signal:
                    c += 1
                    o.seq = c
        handles = {"pe": "tensor", "act": "scalar", "dve": "vector", "pool": "gpsimd", "sp": "sync"}
        stats = {}
        with nc.Block() as block:
            for e in ENGS:
                ops = self.q[e]
                if not ops:
                    continue

                def body(eng, e=e, ops=ops):
                    waited = {}
                    nw = 0
                    for o in ops:
                        wl = []
                        for d in o.deps:
                            if d.is_dma:
                                wl.append((d.dsem, d.dval))
                            else:
                                wl.append((sems[d.eng], d.seq))
                        if o.is_dma and o.dval > 16:
                            wl.append((o.dsem, o.dval - 16))
                        for sem, val in wl:
                            key = id(sem)
                            if waited.get(key, 0) >= val:
                                continue
                            waited[key] = val
                            eng.wait_ge(sem, val)
                            nw += 1
                        if o.fn is None:
                            continue
                        ins = o.fn(eng)
                        if o.is_dma:
                            ins.then_inc(o.dsem, 16)
                        elif o.signal:
                            ins.then_inc(sems[e], 1)
                    for o in ops[::-1]:
                        if o.is_dma:
                            key = id(o.dsem)
                            if waited.get(key, 0) >= o.dval:
                                continue
                            waited[key] = o.dval
                            eng.wait_ge(o.dsem, o.dval)
                    stats[e] = (len(ops), nw)

                getattr(block, handles[e])(body)
        return stats


A0, B0, QI0, KI0, WI0, C0, FG0, D0 = 0, 768, 1536, 1792, 1824, 1832, 2600, 2604


def _swap(cols, d):
    c = cols.reshape(-1, d)
    h = d // 2
    return np.concatenate([c[:, h:], c[:, :h]], axis=1).reshape(-1)


def _inter(c, d):
    s = _swap(c, d)
    return np.concatenate([c[0:128], s[0:128], c[128:256], s[128:256]])


def _win_units():
    ar = np.arange
    u = {}
    u["A_f0"] = A0 + ar(512)
    u["A_t"] = A0 + 512 + ar(256)
    u["C_f0"] = C0 + ar(512)
    u["C_t"] = np.concatenate([C0 + 512 + ar(256), FG0 + ar(4)])
    u["D_f0"] = _inter(D0 + ar(256), 32)
    u["D_f1"] = _inter(D0 + 256 + ar(256), 32)
    u["D_t"] = D0 + 512 + ar(256)
    u["B_f0"] = _inter(B0 + ar(256), 64)
    u["B_f1"] = _inter(B0 + 256 + ar(256), 64)
    u["B_f2"] = _inter(QI0 + ar(256), 32)
    ki4 = np.tile(KI0 + ar(32), 4)
    u["B_f3"] = np.concatenate([ki4, _swap(ki4, 32)])
    u["B_t"] = np.concatenate([B0 + 512 + ar(256), WI0 + ar(8)])
    offs = {}
    o = 0
    cols = []
    for k, v in u.items():
        offs[k] = (o, len(v))
        o += len(v)
        cols.append(v)
    return offs, np.concatenate(cols)


WIN_OFFS, WIN_COLS = _win_units()
NCOL = len(WIN_COLS)


def _rope_tables(d):
    half = d // 2
    inv = (10000.0 ** (-np.arange(0, d, 2, dtype=np.float32) / np.float32(d))).astype(np.float32)
    pos = np.arange(SEQ, dtype=np.float32)
    ang = (pos[:, None] * inv[None, :]).astype(np.float32)
    cos = np.cos(ang).astype(np.float32).T
    sin = np.sin(ang).astype(np.float32).T
    p = np.arange(128)
    j = p % half
    sign = np.where((p % d) < half, -1.0, 1.0).astype(np.float32)
    cs = np.ascontiguousarray(cos[j])
    ss = np.ascontiguousarray(sin[j] * sign[:, None])
    return cs.astype(np.float32), ss.astype(np.float32)


def build_program(n_layers=NL, mixers="ACDB", do_ffn=True, do_final=True, debug=False, nbis=24):
    nc = bass.Bass("TRN2", target_bir_lowering=False)
    dram = lambda name, shape, dt=F32, kind="ExternalInput": nc.dram_tensor(name, list(shape), dt, kind=kind).ap()
    x_d = dram("x", [SEQ, DM])
    win_d = dram("win", [NL, DM, NCOL])
    wo_d = dram("wo", [NL, DM, DM])
    wg_d = dram("wg", [NL, DM, DFF])
    wu_d = dram("wu", [NL, DM, DFF])
    wd_d = dram("wd", [NL, DFF, DM])
    g1_d = dram("g1", [NL, DM])
    g2_d = dram("g2", [NL, DM])
    gf_d = dram("gf", [1, DM])
    relb_d = dram("relb", [NL, 128, 4 * 2 * 128])
    relc_d = dram("relc", [NL, 4])
    fb_d = dram("fb", [NL, 4])
    lam_d = dram("lamv", [NL, 128])
    dng_d = dram("dng", [NL, 64])
    cs64_d = dram("cs64", [128, SEQ])
    ss64_d = dram("ss64", [128, SEQ])
    cs32_d = dram("cs32", [128, SEQ])
    ss32_d = dram("ss32", [128, SEQ])
    out_d = dram("out", [SEQ, DM], F32, "ExternalOutput")
    xres_d = dram("xres", [SEQ, DM], F32, "Internal")
    if debug:
        dbg_h = dram("dbg_h", [128, 8 * SEQ], BF16, "ExternalOutput")
        dbg_m = dram("dbg_m", [128, 8 * SEQ], BF16, "ExternalOutput")
        dbg_x = dram("dbg_x", [SEQ, DM], F32, "ExternalOutput")

    ARENA = 211968
    with ExitStack() as es:
        arena = es.enter_context(nc.sbuf_tensor("arena", [128, ARENA // 4], F32))
        psb = [es.enter_context(nc.psum_tensor(f"ps{i}", [128, 512], F32)) for i in range(8)]
        sems = {e: es.enter_context(nc.semaphore("s_" + e)) for e in ENGS}
        dsems = {e: [es.enter_context(nc.semaphore(f"d_{e}{i}")) for i in range(8)] for e in ENGS}
        S = Sched(nc)

        def carve(off, shape, dt):
            esz = 4 if dt == F32 else 2
            n = int(np.prod(shape[1:]))
            assert off % 4 == 0 and (n * esz) % 4 == 0, (off, shape)
            a = arena[0:shape[0], off // 4: off // 4 + n * esz // 4]
            if dt != F32:
                a = a.bitcast(dt)
            if len(shape) == 3:
                a = a.rearrange("p (a b) -> p a b", a=shape[1])
            elif len(shape) == 4:
                a = a.rearrange("p (a b c) -> p a b c", a=shape[1], b=shape[2])
            return a

        hT = carve(0, [128, 8, SEQ], BF16)
        mixT = carve(32768, [128, 8, SEQ], BF16)
        U = 65536
        X = carve(U, [128, NT, DM], F32)
        QT = carve(U + 0, [128, 2, SEQ], BF16)
        KT = carve(U + 8192, [128, 2, SEQ], BF16)
        V = carve(U + 16384, [128, NT, 4, 128], BF16)
        qiT = carve(U + 32768, [128, 2, SEQ], BF16)
        kiT = carve(U + 40960, [128, SEQ], BF16)
        wiT = carve(U + 45056, [128, NT, 8], F32)
        fgT = carve(U + 45568, [128, NT, 4], F32)
        cumT = carve(U + 45824, [128, NT, 4], F32)
        cbT = carve(U + 46080, [128, NT, 4], F32)
        gexT = carve(U + 46336, [128, NT, 4], F32)
        biasC = carve(U + 46592, [128, 4, 136], F32)
        W = 131072
        wunit = [carve(W + i * 8192, [128, 8, 512], BF16) for i in range(4)]
        hb = [carve(W + 32768 + i * 2048, [128, DM], BF16) for i in range(2)]
        junk = carve(W + 36864, [128, DM], BF16)
        PT = [carve(W + 38912 + i * 1024, [128, 512], BF16) for i in range(4)] + \
             [carve(U + 62592 + i * 1024, [128, 512], BF16) for i in range(2)]
        tmp = [carve(W + 43008 + i * 2048, [128, 512], F32) for i in range(4)]
        obuf = [carve(W + 43008 + i * 4096, [128, DM], F32) for i in range(2)]
        G = carve(W + 51200, [128, DM], F32)
        ident = carve(W + 55296, [128, 128], BF16)
        cst = carve(W + 55296 + 256, [128, 192], F32)
        relbT = carve(W + 56320, [128, 4, 2, 128], F32)
        TAB = [carve(W + 60416 + i * 8192, [128, SEQ], F32) for i in range(2)]
        small2 = carve(W + 76800, [128, 1024], F32)
        assert W + 80896 == ARENA
        scoreb = carve(0, [128, SEQ], F32)
        selb = carve(8192, [128, SEQ], BF16)
        selT = carve(12288, [128, NT, 512], BF16)
        junkb = carve(28672, [128, SEQ], BF16)

        eps_t = cst[:, 0:1]
        relc_t = cst[:, 4:8]
        fb_t = cst[:, 8:12]
        lam_t = cst[:, 12:13]
        nlam_t = cst[:, 13:14]
        lamw = cst[:, 16:48]
        dngT = cst[:, 64:128]
        dngc2 = cst[:, 52:53]
        lamv = small2[:, 0:128]
        tri = small2[:, 128:256]
        mhalf = small2[:, 256:384]
        ones_f = small2[:, 384:512]
        bis = small2[:, 512:640]
        small = small2[:, 640:896]
        identf = tmp[0][:, 0:128]

        T_ps = [Tok(f"ps{i}") for i in range(8)]
        T_hT = [Tok(f"hT{i}") for i in range(NT)]
        T_mix = [[Tok(f"mix{m}{g}") for g in range(4)] for m in range(4)]
        T_X = [Tok(f"X{i}") for i in range(NT)]
        T_QT = [[Tok() for _ in range(4)] for _ in range(2)]
        T_KT = [[Tok() for _ in range(4)] for _ in range(2)]
        T_qi = [[Tok() for _ in range(4)] for _ in range(2)]
        T_ki = [Tok() for _ in range(4)]
        T_V = [Tok() for _ in range(NT)]
        T_wu = [Tok() for _ in range(4)]
        T_hb = [Tok() for _ in range(2)]
        T_PT = [Tok() for _ in range(6)]
        T_tmp = [Tok() for _ in range(4)]
        T_otm = [Tok() for _ in range(2)]
        T_dtmp = Tok()
        T_small = Tok()
        T_G = Tok()
        T_cst = Tok()
        T_tab = Tok()
        T_misc = Tok()
        T_junk = Tok()
        T_ss = [Tok() for _ in range(NT)]
        T_relb = Tok()
        T_score = Tok()
        T_sel = Tok()
        T_selT = [Tok() for _ in range(4)]
        T_bis = Tok()
        T_xres = Tok()
        T_out = Tok()
        T_dbg = Tok()

        def mm(out, lhsT, rhs, start, stop, reads, writes, **kw):
            S.add("pe", lambda e: e.matmul(out, lhsT=lhsT, rhs=rhs, start=start, stop=stop, **kw), reads, writes)

        def tr(out, in_, reads, writes):
            S.add("pe", lambda e: e.transpose(out=out, in_=in_, identity=ident), reads, writes)

        def act(out, in_, func, reads, writes, **kw):
            S.add("act", lambda e: e.activation(out=out, in_=in_, func=func, **kw), reads, writes)

        def ts(eng, out, in0, s1, s2, op0, op1, reads, writes, **kw):
            if op1 is None:
                S.add(eng, lambda e: e.tensor_scalar(out=out, in0=in0, scalar1=s1, scalar2=None, op0=op0, **kw), reads, writes)
            else:
                S.add(eng, lambda e: e.tensor_scalar(out=out, in0=in0, scalar1=s1, scalar2=s2, op0=op0, op1=op1, **kw), reads, writes)

        def tt(eng, out, in0, in1, op, reads, writes):
            S.add(eng, lambda e: e.tensor_tensor(out=out, in0=in0, in1=in1, op=op), reads, writes)

        def stt(out, in0, scalar, in1, op0, op1, reads, writes):
            S.add("dve", lambda e: e.scalar_tensor_tensor(out=out, in0=in0, scalar=scalar, in1=in1, op0=op0, op1=op1), reads, writes)

        def cp(eng, out, in_, reads, writes):
            if eng == "act":
                act(out, in_, AF.Copy, reads, writes)
            else:
                S.add(eng, lambda e: e.tensor_copy(out=out, in_=in_), reads, writes)

        def memset(eng, ap, val, reads, writes):
            S.add(eng, lambda e: e.memset(ap, val), reads, writes)

        def dma(eng, out, in_, reads, writes):
            S.add(eng, lambda e: e.dma_start(out=out, in_=in_), reads, writes, is_dma=True)

        def bcast_rows(src_row_ap, nparts=128):
            return src_row_ap.broadcast_to([nparts, src_row_ap.shape[-1]])

        ps_rr = [0]

        def next_ps():
            i = ps_rr[0] % 8
            ps_rr[0] += 1
            return i

        wlist = []

        def wsrc(d_ap, l, rows0, nrows, c0, ncols):
            return (d_ap[l, rows0:rows0 + nrows, c0:c0 + ncols], nrows // 128, ncols)

        order = [m for m in "ACDB" if m in mixers]
        for l in range(n_layers):
            for m in order:
                names = {"A": ["A_f0", "A_t"], "C": ["C_f0", "C_t"], "D": ["D_f0", "D_f1", "D_t"],
                         "B": ["B_f0", "B_f1", "B_f2", "B_f3", "B_t"]}[m]
                for nm in names:
                    o, n = WIN_OFFS[nm]
                    wlist.append(wsrc(win_d, l, 0, DM, o, n))
            for hf in range(2):
                wlist.append(wsrc(wo_d, l, 0, DM, hf * 512, 512))
            if do_ffn:
                for (f0, nf) in ((0, 1024), (1024, 1024), (2048, 768)):
                    for c0 in range(f0, f0 + nf, 512):
                        nn = min(512, f0 + nf - c0)
                        wlist.append(wsrc(wg_d, l, 0, DM, c0, nn))
                        wlist.append(wsrc(wu_d, l, 0, DM, c0, nn))
                    for hf in range(2):
                        wlist.append(wsrc(wd_d, l, f0, nf, hf * 512, 512))
        wstate = {"next_load": 0, "next_use": 0}

        def w_issue():
            i = wstate["next_load"]
            if i >= len(wlist):
                return
            src, kc, ncols = wlist[i]
            slot = i % 4
            dma("pool", wunit[slot][:, 0:kc, 0:ncols], src.rearrange("(kc p) c -> p kc c", p=128), [], [T_wu[slot]])
            wstate["next_load"] += 1

        def w_get():
            i = wstate["next_use"]
            wstate["next_use"] += 1
            assert i < wstate["next_load"]
            return i % 4

        def w_release():
            w_issue()

        memset("pool", identf, 1.0, [], [T_tmp[0]])
        S.add("pool", lambda e: e.affine_select(out=identf, in_=identf, pattern=[[1, 128]], compare_op=ALU.is_equal,
                                                fill=0.0, base=0, channel_multiplier=-1), [T_tmp[0]], [T_tmp[0]])
        cp("dve", ident, identf, [T_tmp[0]], [T_cst])
        memset("dve", eps_t, EPS, [], [T_cst])
        memset("pool", tri, 1.0, [], [T_misc])
        S.add("pool", lambda e: e.affine_select(out=tri, in_=tri, pattern=[[1, 128]], compare_op=ALU.is_ge,
                                                fill=0.0, base=0, channel_multiplier=-1), [T_misc], [T_misc])
        memset("pool", mhalf, 1.0, [], [T_misc])
        S.add("pool", lambda e: e.affine_select(out=mhalf, in_=mhalf, pattern=[[0, 128]], compare_op=ALU.is_ge,
                                                fill=0.0, base=64, channel_multiplier=-1), [T_misc], [T_misc])
        memset("pool", ones_f, 1.0, [], [T_misc])
        for _ in range(4):
            w_issue()

        def norm_to_hT(l, gsrc):
            dma("sp", G, bcast_rows(gsrc), [], [T_G])
            for tb in range(NT):
                b = tb % 2
                ssq = small[:, tb:tb + 1]
                std = small[:, 16 + tb:17 + tb]
                rstd = small[:, 32 + tb:33 + tb]
                act(junk, X[:, tb, :], AF.Square, [T_X[tb]], [T_junk, T_ss[tb]], accum_out=ssq)
                act(std, ssq, AF.Sqrt, [T_ss[tb], T_cst], [T_ss[tb]], scale=1.0 / DM, bias=eps_t)
                S.add("dve", lambda e, o=rstd, i=std: e.reciprocal(out=o, in_=i), [T_ss[tb]], [T_ss[tb]])
                stt(hb[b], X[:, tb, :], rstd, G, ALU.mult, ALU.mult, [T_X[tb], T_ss[tb], T_G], [T_hb[b]])
                pi = next_ps()
                pT = psb[pi][:, :].bitcast(BF16)
                for kc in range(8):
                    tr(pT[:, kc * 128:(kc + 1) * 128], hb[b][:, kc * 128:(kc + 1) * 128], [T_hb[b], T_cst], [T_ps[pi]])
                cp("act", hT[:, :, tb * 128:(tb + 1) * 128], pT.rearrange("p (a b) -> p a b", a=8), [T_ps[pi]], [T_hT[tb]])

        def proj_feature(slot, nchunks, evac):
            for ci in range(nchunks):
                for tg in range(4):
                    pi = next_ps()
                    for kc in range(8):
                        mm(psb[pi][:, :], wunit[slot][:, kc, ci * 128:(ci + 1) * 128], hT[:, kc, tg * 512:(tg + 1) * 512],
                           kc == 0, kc == 7, [T_wu[slot]] + T_hT[tg * 4:tg * 4 + 4], [T_ps[pi]])
                    evac(ci, tg, pi)

        def proj_feature_pairs(slot, npairs, evac):
            for pj in range(npairs):
                for tg in range(4):
                    pis = []
                    for s in range(2):
                        ci = pj * 2 + s
                        pi = next_ps()
                        pis.append(pi)
                        for kc in range(8):
                            mm(psb[pi][:, :], wunit[slot][:, kc, ci * 128:(ci + 1) * 128], hT[:, kc, tg * 512:(tg + 1) * 512],
                               kc == 0, kc == 7, [T_wu[slot]] + T_hT[tg * 4:tg * 4 + 4], [T_ps[pi]])
                    evac(pj, tg, pis)

        def proj_token(slot, ncols, evac):
            for tb in range(NT):
                pi = next_ps()
                for kc in range(8):
                    mm(psb[pi][:, 0:ncols], hT[:, kc, tb * 128:(tb + 1) * 128], wunit[slot][:, kc, 0:ncols],
                       kc == 0, kc == 7, [T_wu[slot], T_hT[tb]], [T_ps[pi]])
                evac(tb, pi)

        ev_rr = [0]

        def plain_evac(dst_fn, tok_fn):
            def f(ci, tg, pi):
                eng = "act" if ev_rr[0] % 2 == 0 else "dve"
                ev_rr[0] += 1
                cp(eng, dst_fn(ci, tg), psb[pi][:, :], [T_ps[pi]], [tok_fn(ci, tg)])
            return f

        rope_rr = [0]

        def rope_evac(dst_fn, tok_fn):
            def f(pj, tg, pis):
                k = rope_rr[0] % 2
                rope_rr[0] += 1
                t1, t2 = tmp[2 * k], tmp[2 * k + 1]
                tt("dve", t1, psb[pis[0]][:, :], TAB[0][:, tg * 512:(tg + 1) * 512], ALU.mult, [T_ps[pis[0]], T_tab], [T_tmp[2 * k]])
                tt("dve", t2, psb[pis[1]][:, :], TAB[1][:, tg * 512:(tg + 1) * 512], ALU.mult, [T_ps[pis[1]], T_tab], [T_tmp[2 * k + 1]])
                tt("pool", dst_fn(pj, tg), t1, t2, ALU.add, [T_tmp[2 * k], T_tmp[2 * k + 1]], [tok_fn(pj, tg)])
            return f

        def v_evac(extra=None):
            def f(tb, pi):
                eng = "act" if tb % 2 == 0 else "dve"
                cp(eng, V[:, tb, :, 0:64], psb[pi][:, 0:256].rearrange("p (h d) -> p h d", h=4), [T_ps[pi]], [T_V[tb]])
                if extra is not None:
                    extra(tb, pi)
            return f

        def load_tables(d):
            a, b = (cs64_d, ss64_d) if d == 64 else (cs32_d, ss32_d)
            dma("sp", TAB[0], a, [], [T_tab])
            dma("sp", TAB[1], b, [], [T_tab])

        st_rr = [0]
        op_rr = [0]
        pt_rr = [0]
        ST_BANKS = [0, 1, 2]
        OP_BANKS = [3, 4, 5, 6]
        TR_BANK = 7

        pend = []
        SKEW = 2

        def pipe_push(fn, skew=SKEW):
            pend.append([fn, []])
            while len(pend) > skew:
                f, cbs = pend.pop(0)
                f()
                for c in cbs:
                    c()

        def pipe_cb(fn):
            if pend:
                pend[-1][1].append(fn)
            else:
                fn()

        def pipe_flush():
            while pend:
                f, cbs = pend.pop(0)
                f()
                for c in cbs:
                    c()

        st_banks = {"l": [0, 1, 2]}

        def attn_group(m, g, kbs, vheads):
            obs = []
            for _ in vheads:
                obs.append(OP_BANKS[op_rr[0] % 4])
                op_rr[0] += 1
            SKEW_ = len(vheads) * (2 if len(vheads) <= 2 else 1)
            first = True
            for kb in kbs:
                if m == "A":
                    qbs = [qb for qb in range(4 * g, 4 * g + 4) if kb <= qb <= kb + 4]
                else:
                    qbs = [qb for qb in range(4 * g, 4 * g + 4) if qb >= kb]
                if not qbs:
                    continue
                n = len(qbs) * 128
                q0 = qbs[0] * 128
                for vi, vhd in enumerate(vheads):
                    stl = st_banks["l"]
                    sb_i = stl[st_rr[0] % len(stl)]
                    st_rr[0] += 1
                    pt_i = pt_rr[0] % 6
                    pt_rr[0] += 1
                    base, rows, tile = vhd["base"], vhd["rows"], vhd["tile"]
                    lhsT = KT[base:base + rows, tile, kb * 128:(kb + 1) * 128]
                    rhs = QT[base:base + rows, tile, q0:q0 + n]
                    kw = {}
                    if base == 96:
                        kw["tile_position"] = (96, 0)
                    mm(psb[sb_i][:, 0:n], lhsT, rhs, True, True, [vhd["qt"][g], vhd["kt"][kb // 4]], [T_ps[sb_i]], **kw)
                    vhd["exp"](kb, qbs, sb_i, pt_i)
                    vhd["post"](kb, qbs, pt_i)

                    def pv(kb=kb, qbs=qbs, pt_i=pt_i, first=first, n=n, ob=obs[vi], vh=vhd["vh"]):
                        c0 = (qbs[0] - 4 * g) * 128
                        mm(psb[ob][:, c0:c0 + n], V[:, kb, vh, :], PT[pt_i][:, 0:n],
                           first, False, [T_PT[pt_i], T_V[kb]], [T_ps[ob]], skip_group_check=True)
                    pipe_push(pv, SKEW_)
                    bg_tick()
                first = False
            return obs

        NB = TR_BANK
        bg = {"chunks": [], "per": 0}

        def bg_tick():
            for _ in range(bg["per"]):
                if bg["chunks"]:
                    bg["chunks"].pop(0)()

        def bg_drain():
            while bg["chunks"]:
                bg["chunks"].pop(0)()
            bg["per"] = 0

        def norm_fm(ob, mi, h, g):
            k = rope_rr[0] % 4
            rope_rr[0] += 1
            act(tmp[k][0:64, :], psb[ob][64:128, :], AF.Ln, [T_ps[ob]], [T_tmp[k]])
            act(tmp[k][0:64, :], tmp[k][0:64, :], AF.Exp, [T_tmp[k]], [T_tmp[k]], scale=-1.0)
            r0 = (h % 2) * 64
            tt("dve", mixT[r0:r0 + 64, 2 * mi + h // 2, g * 512:(g + 1) * 512], psb[ob][0:64, :], tmp[k][0:64, :], ALU.mult,
               [T_ps[ob], T_tmp[k]], [T_mix[mi][g]])

        T_tmpX = [[Tok() for _ in range(4)] for _ in range(2)]

        def d_fork():
            for k in range(4):
                memset("pool", tmp[k][0:1, 0:1], 0.0, [T_tmp[k]], [T_tmp[k], T_tmpX[0][k], T_tmpX[1][k]])

        def d_join():
            for k in range(4):
                memset("pool", tmp[k][0:1, 0:1], 0.0, [T_tmpX[0][k], T_tmpX[1][k]], [T_tmp[k]])

        def d_combine_fm(obs, mi, h, g):
            st_ = h % 2
            p0 = st_ * 64
            tk = T_tmpX[st_]
            t0, t1, t2, t3 = [t[p0:p0 + 64, :] for t in tmp]
            o1 = psb[obs[0]]
            o2 = psb[obs[1]]
            act(t0, o1[64:128, :], AF.Ln, [T_ps[obs[0]]], [tk[0]])
            act(t0, t0, AF.Exp, [tk[0]], [tk[0]], scale=-1.0)
            act(t1, o2[64:128, :], AF.Ln, [T_ps[obs[1]]], [tk[1]])
            act(t1, t1, AF.Exp, [tk[1]], [tk[1]], scale=-1.0)
            tt("dve", t2, o1[0:64, :], t0, ALU.mult, [T_ps[obs[0]], tk[0]], [tk[2]])
            tt("dve", t3, o2[0:64, :], t1, ALU.mult, [T_ps[obs[1]], tk[1]], [tk[3]])
            stt(t2, t3, nlam_t[p0:p0 + 64, :], t2, ALU.mult, ALU.add, [tk[3], tk[2], T_cst], [tk[2]])
            tt("pool", t3, t2, t2, ALU.mult, [tk[2]], [tk[3]])
            mm(psb[NB][0:64, :], ones_f[p0:p0 + 64, 0:64], t3, True, True, [tk[3], T_misc], [T_ps[NB]])
            act(t0, psb[NB][0:64, :], AF.Ln, [T_ps[NB], T_cst], [tk[0]], scale=1.0 / 64, bias=eps_t[p0:p0 + 64, :])
            act(t1, t0, AF.Exp, [tk[0]], [tk[1]], scale=-0.5)
            r0 = (h % 2) * 64
            stt(mixT[r0:r0 + 64, 2 * mi + h // 2, g * 512:(g + 1) * 512], t2, dngc2[p0:p0 + 64, :], t1, ALU.mult, ALU.mult,
                [tk[2], tk[1], T_cst], [T_mix[mi][g]])

        otm_rr = [0]

        for l in range(n_layers):
            lam_init = 0.8 - 0.6 * math.exp(-0.3 * l)
            if l == 0:
                for tb in range(NT):
                    dma("sp", X[:, tb, :], x_d[tb * 128:(tb + 1) * 128, :], [], [T_X[tb]])
            dma("sp", relc_t, bcast_rows(relc_d[l:l + 1, :]), [], [T_cst])
            dma("sp", fb_t, bcast_rows(fb_d[l:l + 1, :]), [], [T_cst])
            dma("sp", lamv, bcast_rows(lam_d[l:l + 1, :]), [], [T_misc])
            dma("sp", dngT, bcast_rows(dng_d[l:l + 1, :]), [], [T_cst])
            dma("sp", relbT, relb_d[l].rearrange("k (h d q) -> k h d q", h=4, d=2), [], [T_relb])
            tt("dve", lamw, lamv[:, 0:32], lamv[:, 32:64], ALU.mult, [T_misc], [T_cst])
            S.add("dve", lambda e: e.tensor_reduce(out=cst[:, 48:49], in_=lamw, axis=AX.X, op=ALU.add), [T_cst], [T_cst])
            tt("dve", lamw, lamv[:, 64:96], lamv[:, 96:128], ALU.mult, [T_misc, T_cst], [T_cst])
            S.add("dve", lambda e: e.tensor_reduce(out=cst[:, 49:50], in_=lamw, axis=AX.X, op=ALU.add), [T_cst], [T_cst])
            act(cst[:, 50:52], cst[:, 48:50], AF.Exp, [T_cst], [T_cst])
            tt("dve", lam_t, cst[:, 50:51], cst[:, 51:52], ALU.subtract, [T_cst], [T_cst])
            ts("dve", lam_t, lam_t, lam_init, None, ALU.add, None, [T_cst], [T_cst])
            ts("dve", nlam_t, lam_t, -1.0, None, ALU.mult, None, [T_cst], [T_cst])
            ts("dve", dngT, dngT, 1.0 - lam_init, None, ALU.mult, None, [T_cst], [T_cst])
            dma("sp", dngc2[0:64, :], dng_d[l].rearrange("(d o) -> d o", o=1), [], [T_cst])
            dma("sp", dngc2[64:128, :], dng_d[l].rearrange("(d o) -> d o", o=1), [], [T_cst])
            ts("dve", dngc2, dngc2, 1.0 - lam_init, None, ALU.mult, None, [T_cst], [T_cst])

            norm_to_hT(l, g1_d[l:l + 1, :])
            if l > 0:
                for tb in range(NT):
                    dma("sp", xres_d[tb * 128:(tb + 1) * 128, :], X[:, tb, :], [T_X[tb]], [T_xres])
            if debug and l == 0:
                dma("sp", dbg_h, hT.rearrange("p a b -> p (a b)"), T_hT, [T_dbg])
            S.barrier()
            memset("pool", V[:, :, :, 64:128], 1.0, [], T_V)

            for mi_, m in enumerate(order):
                mi = "ABCD".index(m)
                if m in "AC":
                    s0 = w_get()
                    proj_feature(s0, 4, plain_evac(
                        lambda ci, tg: (QT if ci < 2 else KT)[:, ci % 2, tg * 512:(tg + 1) * 512],
                        lambda ci, tg: (T_QT if ci < 2 else T_KT)[ci % 2][tg]))
                    w_release()
                    s1 = w_get()
                    if m == "A":
                        proj_token(s1, 256, v_evac())
                    else:
                        def fg_extra(tb, pi):
                            cp("dve", fgT[:, tb, :], psb[pi][:, 256:260], [T_ps[pi]], [T_misc])
                        proj_token(s1, 260, v_evac(fg_extra))
                    w_release()
                elif m == "D":
                    load_tables(32)
                    for (dst, dtk) in ((QT, T_QT), (KT, T_KT)):
                        s0 = w_get()
                        proj_feature_pairs(s0, 2, rope_evac(
                            lambda pj, tg, dst=dst: dst[:, pj, tg * 512:(tg + 1) * 512],
                            lambda pj, tg, dtk=dtk: dtk[pj][tg]))
                        w_release()
                    s1 = w_get()
                    proj_token(s1, 256, v_evac())
                    w_release()
                elif m == "B":
                    load_tables(64)
                    for (dst, dtk) in ((QT, T_QT), (KT, T_KT)):
                        s0 = w_get()
                        proj_feature_pairs(s0, 2, rope_evac(
                            lambda pj, tg, dst=dst: dst[:, pj, tg * 512:(tg + 1) * 512],
                            lambda pj, tg, dtk=dtk: dtk[pj][tg]))
                        w_release()
                    load_tables(32)
                    s0 = w_get()
                    proj_feature_pairs(s0, 2, rope_evac(
                        lambda pj, tg: qiT[:, pj, tg * 512:(tg + 1) * 512],
                        lambda pj, tg: T_qi[pj][tg]))
                    w_release()
                    s0 = w_get()
                    proj_feature_pairs(s0, 1, rope_evac(
                        lambda pj, tg: kiT[:, tg * 512:(tg + 1) * 512],
                        lambda pj, tg: T_ki[tg]))
                    w_release()
                    s1 = w_get()

                    def wi_extra(tb, pi):
                        cp("dve", wiT[:, tb, :], psb[pi][:, 256:264], [T_ps[pi]], [T_misc])
                    proj_token(s1, 264, v_evac(wi_extra))
                    w_release()

                if m == "A":
                    def a_exp(h):
                        def f(kb, qbs, sb_i, pt_i):
                            far = [qb for qb in qbs if qb - kb >= 2]
                            for j, qb in enumerate(qbs):
                                dl = qb - kb
                                if dl <= 1:
                                    k = rope_rr[0] % 4
                                    rope_rr[0] += 1
                                    stt(tmp[k][:, 0:128], psb[sb_i][:, j * 128:(j + 1) * 128], 0.125, relbT[:, h, dl, :],
                                        ALU.mult, ALU.add, [T_ps[sb_i], T_relb], [T_tmp[k]])
                                    act(PT[pt_i][:, j * 128:(j + 1) * 128], tmp[k][:, 0:128], AF.Exp, [T_tmp[k]], [T_PT[pt_i]])
                            if far:
                                j0 = qbs.index(far[0])
                                act(PT[pt_i][:, j0 * 128:(j0 + len(far)) * 128], psb[sb_i][:, j0 * 128:(j0 + len(far)) * 128], AF.Exp,
                                    [T_ps[sb_i], T_cst], [T_PT[pt_i]], scale=0.125, bias=relc_t[:, h:h + 1])
                        return f

                    def a_post(kb, qbs, pt_i):
                        for j, qb in enumerate(qbs):
                            if qb == kb:
                                memset("pool", PT[pt_i][64:128, j * 128:j * 128 + 64], 0.0, [T_PT[pt_i]], [T_PT[pt_i]])
                            if qb == kb + 4:
                                memset("pool", PT[pt_i][0:64, j * 128 + 64:j * 128 + 128], 0.0, [T_PT[pt_i]], [T_PT[pt_i]])

                    st_banks["l"] = [0, 1, 2, 7]
                    for g in range(4):
                        kbs = list(range(max(0, 4 * g - 4), 4 * g + 4))
                        for hp in range(2):
                            vhs = [dict(tile=hp, base=(h % 2) * 64, rows=64, vh=h, exp=a_exp(h), post=a_post, qt=T_QT[hp], kt=T_KT[hp])
                                   for h in (2 * hp, 2 * hp + 1)]
                            obs = attn_group("A", g, kbs, vhs)
                            for ob, h in zip(obs, (2 * hp, 2 * hp + 1)):
                                pipe_cb(lambda ob=ob, h=h, g=g: norm_fm(ob, mi, h, g))
                    pipe_flush()
                    st_banks["l"] = [0, 1, 2]

                elif m == "C":
                    fg2 = fgT.rearrange("p a b -> p (a b)")
                    tt("dve", fgT, fgT, fb_t.unsqueeze(1).to_broadcast([128, NT, 4]), ALU.add, [T_misc, T_cst], [T_misc])
                    act(fg2, fg2, AF.Exp, [T_misc], [T_misc], scale=-1.0)
                    act(fg2, fg2, AF.Ln, [T_misc], [T_misc], bias=1.0)
                    ts("dve", fg2, fg2, -1.0, None, ALU.mult, None, [T_misc], [T_misc])
                    memset("dve", gexT[:, 0, :], 0.0, [], [T_misc])
                    for tb in range(1, NT):
                        tt("dve", gexT[:, tb, :], gexT[:, tb - 1, :], fgT[:, tb - 1, :], ALU.add, [T_misc], [T_misc])
                    pi = next_ps()
                    pj = next_ps()
                    gex2 = gexT.rearrange("p a b -> p (a b)")
                    mm(psb[pi][:, 0:64], tri, fg2, True, False, [T_misc], [T_ps[pi]])
                    mm(psb[pi][:, 0:64], ones_f, gex2, False, True, [T_misc], [T_ps[pi]])
                    mm(psb[pj][:, 0:64], mhalf, fg2, True, False, [T_misc], [T_ps[pj]])
                    mm(psb[pj][:, 0:64], ones_f, gex2, False, True, [T_misc], [T_ps[pj]])
                    cp("dve", cumT.rearrange("p a b -> p (a b)"), psb[pi][:, 0:64], [T_ps[pi]], [T_misc])
                    cp("dve", cbT.rearrange("p a b -> p (a b)"), psb[pj][:, 0:64], [T_ps[pj]], [T_misc])
                    pair_idx = {}
                    pidx = 0
                    for qb in range(NT):
                        for h in range(4):
                            ts("dve", biasC[:, h, pidx:pidx + qb + 1], cumT[:, 0:qb + 1, h], -1.0, cbT[:, qb, h:h + 1],
                               ALU.mult, ALU.add, [T_misc], [T_misc])
                        for kb in range(qb + 1):
                            pair_idx[(qb, kb)] = pidx + kb
                        pidx += qb + 1

                    def c_exp(h):
                        def f(kb, qbs, sb_i, pt_i):
                            for j, qb in enumerate(qbs):
                                act(PT[pt_i][:, j * 128:(j + 1) * 128], psb[sb_i][:, j * 128:(j + 1) * 128], AF.Exp,
                                    [T_ps[sb_i], T_misc], [T_PT[pt_i]], scale=0.125,
                                    bias=biasC[:, h, pair_idx[(qb, kb)]:pair_idx[(qb, kb)] + 1])
                        return f

                    def c_post(kb, qbs, pt_i):
                        for j, qb in enumerate(qbs):
                            if qb == kb:
                                ap = PT[pt_i][:, j * 128:(j + 1) * 128]
                                S.add("pool", lambda e, ap=ap: e.affine_select(out=ap, in_=ap, pattern=[[1, 128]], compare_op=ALU.is_ge,
                                                                               fill=0.0, base=0, channel_multiplier=-1),
                                      [T_PT[pt_i]], [T_PT[pt_i]])

                    st_banks["l"] = [0, 1, 2, 7]
                    for g in range(4):
                        for hp in range(2):
                            vhs = [dict(tile=hp, base=(h % 2) * 64, rows=64, vh=h, exp=c_exp(h), post=c_post, qt=T_QT[hp], kt=T_KT[hp])
                                   for h in (2 * hp, 2 * hp + 1)]
                            obs = attn_group("C", g, list(range(4 * g + 4)), vhs)
                            for ob, h in zip(obs, (2 * hp, 2 * hp + 1)):
                                pipe_cb(lambda ob=ob, h=h, g=g: norm_fm(ob, mi, h, g))
                    pipe_flush()
                    st_banks["l"] = [0, 1, 2]

                elif m in "DB":
                    sc = 32 ** -0.5 if m == "D" else 0.125

                    def d_exp(kb, qbs, sb_i, pt_i):
                        n = len(qbs) * 128
                        act(PT[pt_i][:, 0:n], psb[sb_i][:, 0:n], AF.Exp, [T_ps[sb_i]], [T_PT[pt_i]], scale=sc)

                    def d_post(kb, qbs, pt_i):
                        for j, qb in enumerate(qbs):
                            if qb == kb:
                                memset("pool", PT[pt_i][64:128, j * 128:j * 128 + 64], 0.0, [T_PT[pt_i]], [T_PT[pt_i]])

                    if m == "D":
                        d_fork()
                        for g in range(4):
                            for h in range(4):
                                hp = h // 2
                                vhs = [dict(tile=hp, base=((h % 2) * 2 + r) * 32, rows=32, vh=h, exp=d_exp, post=d_post,
                                            qt=T_QT[hp], kt=T_KT[hp]) for r in range(2)]
                                obs = attn_group("D", g, list(range(4 * g + 4)), vhs)
                                pipe_cb(lambda obs=obs, h=h, g=g: d_combine_fm(obs, mi, h, g))
                        pipe_flush()
                        d_join()
                    else:
                        S.barrier()
                        KSEL = 256.0
                        selTs = [carve(0, [128, 12, 512], BF16), carve(12288, [128, 16, 512], BF16)]
                        junkb2 = carve(28672, [128, SEQ], BF16)
                        uleft = carve(U + 48768, [128, 3456], F32)
                        scb = [uleft[:, 1792:3456], uleft[:, 0:1792], TAB[1], TAB[0]]
                        selb2 = carve(W + 32768, [128, SEQ], BF16)
                        T_sc = [Tok() for _ in range(4)]
                        T_bq = [Tok() for _ in range(4)]
                        T_sT = [[Tok() for _ in range(4)] for _ in range(2)]
                        T_selb = Tok()

                        ajunk = G.bitcast(BF16)
                        ACT_CHAINS = (0, 1)

                        def b_select_chunks(g):
                            chunks = []
                            sT = selTs[g % 2]
                            tks = T_sT[g % 2]
                            active = []
                            for jq in range(4):
                                qb = 4 * g + jq
                                if qb < 2:
                                    chunks.append(lambda jq=jq, qb=qb: memset("pool", sT[:, 0:qb + 1, jq * 128:(jq + 1) * 128], 1.0, [], [tks[jq]]))
                                else:
                                    active.append((jq, qb))

                            def score_piece(jq, qb, gi, c0, n):
                                base = (gi % 4) * 32
                                kw = {"tile_position": (96, 0)} if base == 96 else {}
                                sb_i = ST_BANKS[st_rr[0] % 3]
                                st_rr[0] += 1
                                k = rope_rr[0] % 4
                                rope_rr[0] += 1
                                mm(psb[sb_i][:, 0:n], qiT[base:base + 32, gi // 4, qb * 128:(qb + 1) * 128],
                                   kiT[base:base + 32, c0:c0 + n], True, True,
                                   [T_qi[gi // 4][g]] + T_ki, [T_ps[sb_i]], **kw)
                                act(tmp[k][:, 0:n], psb[sb_i][:, 0:n], AF.Relu, [T_ps[sb_i]], [T_tmp[k]])
                                if gi == 0:
                                    ts("dve", scb[jq][:, c0:c0 + n], tmp[k][:, 0:n], wiT[:, qb, 0:1], None, ALU.mult, None,
                                       [T_tmp[k], T_misc], [T_sc[jq]])
                                else:
                                    stt(scb[jq][:, c0:c0 + n], tmp[k][:, 0:n], wiT[:, qb, gi:gi + 1], scb[jq][:, c0:c0 + n],
                                        ALU.mult, ALU.add, [T_tmp[k], T_misc, T_sc[jq]], [T_sc[jq]])

                            for jq, qb in active:
                                nk = (qb + 1) * 128
                                for gi in range(8):
                                    for c0 in range(0, nk, 512):
                                        n = min(512, nk - c0)
                                        chunks.append(lambda jq=jq, qb=qb, gi=gi, c0=c0, n=n: score_piece(jq, qb, gi, c0, n))
                                chunks.append(lambda jq=jq, qb=qb: memset("dve", scb[jq][0:64, qb * 128 + 64:qb * 128 + 128], -1.0e30,
                                                                          [T_sc[jq]], [T_sc[jq]]))

                            def init_piece():
                                for jq, qb in active:
                                    nk = (qb + 1) * 128
                                    memset("pool", bis[:, 4 * jq:4 * jq + 1], 0.0, [T_bq[jq]], [T_bq[jq]])
                                    if jq in ACT_CHAINS:
                                        memset("pool", bis[:, 4 * jq + 3:4 * jq + 4], float(nk) - 2.0 * KSEL + 0.5, [T_bq[jq]], [T_bq[jq]])
                            chunks.append(init_piece)

                            def round_piece(step):
                                for jq, qb in active:
                                    nk = (qb + 1) * 128
                                    c = 4 * jq
                                    if jq in ACT_CHAINS:
                                        act(ajunk[:, 0:nk], scb[jq][:, 0:nk], AF.Sign, [T_sc[jq], T_bq[jq]], [T_bq[jq]],
                                            bias=bis[:, c:c + 1], accum_out=bis[:, c + 1:c + 2])
                                    else:
                                        ts("dve", junkb2[:, 0:nk], scb[jq][:, 0:nk], bis[:, c:c + 1], None, ALU.is_ge, ALU.add,
                                           [T_sc[jq], T_bq[jq]], [T_bq[jq]], accum_out=bis[:, c + 1:c + 2])
                                for jq, qb in active:
                                    c = 4 * jq
                                    if jq in ACT_CHAINS:
                                        act(bis[:, c + 2:c + 3], bis[:, c + 1:c + 2], AF.Sign, [T_bq[jq]], [T_bq[jq]], bias=bis[:, c + 3:c + 4])
                                    else:
                                        ts("dve", bis[:, c + 2:c + 3], bis[:, c + 1:c + 2], KSEL, 2.0 * step,
                                           ALU.is_ge, ALU.mult, [T_bq[jq]], [T_bq[jq]])
                                for jq, qb in active:
                                    c = 4 * jq
                                    if jq in ACT_CHAINS:
                                        act(bis[:, c:c + 1], bis[:, c + 2:c + 3], AF.Identity, [T_bq[jq]], [T_bq[jq]],
                                            scale=-step, bias=bis[:, c:c + 1])
                                    else:
                                        stt(bis[:, c:c + 1], bis[:, c + 2:c + 3], -step, bis[:, c:c + 1],
                                            ALU.add, ALU.add, [T_bq[jq]], [T_bq[jq]])

                            step = 64.0
                            for it in range(nbis):
                                chunks.append(lambda step=step: round_piece(step))
                                step *= 0.5
                            fstep = step

                            def final_piece(jq, qb):
                                nk = (qb + 1) * 128
                                mid = bis[:, 4 * jq:4 * jq + 1]
                                if jq in ACT_CHAINS:
                                    ts("dve", mid, mid, -1.0, -2.0 * fstep, ALU.mult, ALU.add, [T_bq[jq]], [T_bq[jq]])
                                else:
                                    ts("dve", mid, mid, -2.0 * fstep, None, ALU.add, None, [T_bq[jq]], [T_bq[jq]])
                                ts("dve", selb2[:, 0:nk], scb[jq][:, 0:nk], mid, None, ALU.is_ge, None, [T_sc[jq], T_bq[jq]], [T_selb])
                                for k0 in range(0, qb + 1, 4):
                                    kn = min(4, qb + 1 - k0)
                                    pT = psb[TR_BANK][:, :].bitcast(BF16)
                                    for kk in range(kn):
                                        tr(pT[:, kk * 128:(kk + 1) * 128], selb2[:, (k0 + kk) * 128:(k0 + kk + 1) * 128],
                                           [T_selb, T_cst], [T_ps[TR_BANK]])
                                    cp("act", sT[:, k0:k0 + kn, jq * 128:(jq + 1) * 128],
                                       pT[:, 0:kn * 128].rearrange("p (a b) -> p a b", a=kn), [T_ps[TR_BANK]], [tks[jq]])
                            for jq, qb in active:
                                chunks.append(lambda jq=jq, qb=qb: final_piece(jq, qb))
                            return chunks

                        def b_attend(g):
                            sT = selTs[g % 2]
                            tks = T_sT[g % 2]

                            def b_post(kb, qbs, pt_i):
                                n = len(qbs) * 128
                                j0 = qbs[0] - 4 * g
                                for j, qb in enumerate(qbs):
                                    if qb == kb:
                                        memset("pool", PT[pt_i][64:128, j * 128:j * 128 + 64], 0.0, [T_PT[pt_i]], [T_PT[pt_i]])
                                tt("pool", PT[pt_i][:, 0:n], PT[pt_i][:, 0:n], sT[:, kb, j0 * 128:j0 * 128 + n], ALU.mult,
                                   [T_PT[pt_i]] + tks, [T_PT[pt_i]])

                            for hp in range(2):
                                vhs = [dict(tile=hp, base=(h % 2) * 64, rows=64, vh=h, exp=d_exp, post=b_post, qt=T_QT[hp], kt=T_KT[hp])
                                       for h in (2 * hp, 2 * hp + 1)]
                                obs = attn_group("B", g, list(range(4 * g + 4)), vhs)
                                for ob, h in zip(obs, (2 * hp, 2 * hp + 1)):
                                    pipe_cb(lambda ob=ob, h=h, g=g: norm_fm(ob, mi, h, g))
                            pipe_flush()

                        for c in b_select_chunks(0):
                            c()
                        for g in range(4):
                            if g + 1 < 4:
                                bg["chunks"] = b_select_chunks(g + 1)
                                nsteps = 4 * (4 * g + 4)
                                bg["per"] = -(-len(bg["chunks"]) // nsteps)
                            b_attend(g)
                            bg_drain()

            if debug and l == 0:
                S.barrier()
                dma("sp", dbg_m, mixT.rearrange("p a b -> p (a b)"), [t for tt_ in T_mix for t in tt_], [T_dbg])

            S.barrier()
            xsrc = x_d if l == 0 else xres_d
            for tb in range(NT):
                dma("sp", X[:, tb, :], xsrc[tb * 128:(tb + 1) * 128, :], [T_xres], [T_X[tb]])
            s0 = w_get()
            s1 = w_get()
            for tb in range(NT):
                for hf, sl in enumerate((s0, s1)):
                    pi = next_ps()
                    for kc in range(8):
                        mm(psb[pi][:, :], mixT[:, kc, tb * 128:(tb + 1) * 128], wunit[sl][:, kc, :], kc == 0, kc == 7,
                           [T_wu[sl], T_mix[kc // 2][tb // 4]], [T_ps[pi]])
                    tt("dve", X[:, tb, hf * 512:(hf + 1) * 512], X[:, tb, hf * 512:(hf + 1) * 512], psb[pi][:, :], ALU.add,
                       [T_ps[pi], T_X[tb]], [T_X[tb]])
            w_release()
            w_release()
            if debug and l == 0 and not do_ffn:
                for tb in range(NT):
                    dma("sp", dbg_x[tb * 128:(tb + 1) * 128, :], X[:, tb, :], [T_X[tb]], [T_dbg])

            if do_ffn:
                norm_to_hT(l, g2_d[l:l + 1, :])
                actT = mixT
                T_act = [Tok() for _ in range(NT)]
                for (f0, nf) in ((0, 1024), (1024, 1024), (2048, 768)):
                    nfc = nf // 128
                    fc = 0
                    for c0 in range(f0, f0 + nf, 512):
                        nn = min(512, f0 + nf - c0)
                        sg = w_get()
                        su = w_get()
                        for ci in range(nn // 128):
                            for tg in range(4):
                                pg = next_ps()
                                pu = next_ps()
                                for kc in range(8):
                                    mm(psb[pg][:, :], wunit[sg][:, kc, ci * 128:(ci + 1) * 128], hT[:, kc, tg * 512:(tg + 1) * 512],
                                       kc == 0, kc == 7, [T_wu[sg]] + T_hT[tg * 4:tg * 4 + 4], [T_ps[pg]])
                                for kc in range(8):
                                    mm(psb[pu][:, :], wunit[su][:, kc, ci * 128:(ci + 1) * 128], hT[:, kc, tg * 512:(tg + 1) * 512],
                                       kc == 0, kc == 7, [T_wu[su]] + T_hT[tg * 4:tg * 4 + 4], [T_ps[pu]])
                                k = rope_rr[0] % 4
                                rope_rr[0] += 1
                                act(tmp[k], psb[pg][:, :], AF.Silu, [T_ps[pg]], [T_tmp[k]])
                                tt("dve", actT[:, fc, tg * 512:(tg + 1) * 512], psb[pu][:, :], tmp[k], ALU.mult,
                                   [T_ps[pu], T_tmp[k]], T_act[tg * 4:tg * 4 + 4])
                            fc += 1
                        w_release()
                        w_release()
                    for hf in range(2):
                        sd = w_get()
                        for tb in range(NT):
                            pi = next_ps()
                            for k2 in range(nfc):
                                mm(psb[pi][:, :], actT[:, k2, tb * 128:(tb + 1) * 128], wunit[sd][:, k2, :], k2 == 0, k2 == nfc - 1,
                                   [T_wu[sd], T_act[tb]], [T_ps[pi]])
                            tt("dve", X[:, tb, hf * 512:(hf + 1) * 512], X[:, tb, hf * 512:(hf + 1) * 512], psb[pi][:, :], ALU.add,
                               [T_ps[pi], T_X[tb]], [T_X[tb]])
                        w_release()
                if debug and l == 0:
                    for tb in range(NT):
                        dma("sp", dbg_x[tb * 128:(tb + 1) * 128, :], X[:, tb, :], [T_X[tb]], [T_dbg])

        if do_final:
            S.barrier()
            dma("sp", G, bcast_rows(gf_d[0:1, :]), [], [T_G])
            T_ob = [Tok(), Tok()]
            for tb in range(NT):
                b = tb % 2
                ssq = small[:, tb:tb + 1]
                std = small[:, 16 + tb:17 + tb]
                rstd = small[:, 32 + tb:33 + tb]
                act(junk, X[:, tb, :], AF.Square, [T_X[tb]], [T_junk, T_ss[tb]], accum_out=ssq)
                act(std, ssq, AF.Sqrt, [T_ss[tb], T_cst], [T_ss[tb]], scale=1.0 / DM, bias=eps_t)
                S.add("dve", lambda e, o=rstd, i=std: e.reciprocal(out=o, in_=i), [T_ss[tb]], [T_ss[tb]])
                stt(obuf[b], X[:, tb, :], rstd, G, ALU.mult, ALU.mult, [T_X[tb], T_ss[tb], T_G], [T_ob[b]])
                dma("sp", out_d[tb * 128:(tb + 1) * 128, :], obuf[b], [T_ob[b]], [T_out])
        else:
            for tb in range(NT):
                dma("sp", out_d[tb * 128:(tb + 1) * 128, :], X[:, tb, :], [T_X[tb]], [T_out])

        stats = S.emit(sems, dsems)
    return nc, stats


def _prep_shared(ln1_g, w_in, rel_bias, forget_b, lam_q1, lam_k1, lam_q2, lam_k2, diff_norm_g, w_o, ln2_g,
                 w_gate, w_up, w_down, final_g):
    f = lambda a: np.ascontiguousarray(np.asarray(a, dtype=np.float32))
    sh = {}
    sh["win"] = f(np.asarray(w_in)[:, :, WIN_COLS])
    sh["wo"] = f(w_o)
    sh["wg"] = f(w_gate)
    sh["wu"] = f(w_up)
    sh["wd"] = f(w_down)
    sh["g1"] = f(ln1_g)
    sh["g2"] = f(ln2_g)
    sh["gf"] = f(np.asarray(final_g).reshape(1, DM))
    rb = np.asarray(rel_bias, dtype=np.float32)
    k = np.arange(128)[:, None]
    q = np.arange(128)[None, :]
    tiles = []
    for dl in range(2):
        idx = np.clip(q - k + 128 * dl, -128, 128) + 128
        tiles.append(rb[:, :, idx])
    t = np.stack(tiles, axis=2)
    sh["relb"] = f(np.transpose(t, (0, 3, 1, 2, 4)).reshape(NL, 128, 4 * 2 * 128))
    sh["relc"] = f(rb[:, :, 256])
    sh["fb"] = f(forget_b)
    sh["lamv"] = f(np.concatenate([np.asarray(lam_q1), np.asarray(lam_k1), np.asarray(lam_q2), np.asarray(lam_k2)], axis=1))
    sh["dng"] = f(diff_norm_g)
    sh["cs64"], sh["ss64"] = _rope_tables(64)
    sh["cs32"], sh["ss32"] = _rope_tables(32)
    return sh


_CACHE = {}


def kernel(x, ln1_g, w_in, rel_bias, forget_b, lam_q1, lam_k1, lam_q2, lam_k2, diff_norm_g, w_o, ln2_g,
           w_gate, w_up, w_down, final_g):
    x = np.asarray(x, dtype=np.float32)
    sh = _prep_shared(ln1_g, w_in, rel_bias, forget_b, lam_q1, lam_k1, lam_q2, lam_k2, diff_norm_g, w_o, ln2_g,
                      w_gate, w_up, w_down, final_g)
    if "nc" not in _CACHE:
        _CACHE["nc"] = build_program()[0]
    nc = _CACHE["nc"]
    in_maps = []
    for c in range(NCORES):
        mp = dict(sh)
        mp["x"] = np.ascontiguousarray(x[c])
        in_maps.append(mp)
    res = run_bass_kernel_spmd(nc, in_maps, core_ids=list(range(NCORES)))
    out = np.stack([np.asarray(res.results[c]["out"], dtype=np.float32).reshape(SEQ, DM) for c in range(NCORES)], axis=0)
    return out
```

```python
import math
from contextlib import ExitStack

import numpy as np
import concourse.bass as bass
import concourse.mybir as mybir
from concourse.bass_utils import run_bass_kernel_spmd

F32 = mybir.dt.float32
BF16 = mybir.dt.bfloat16
AF = mybir.ActivationFunctionType
ALU = mybir.AluOpType
AX = mybir.AxisListType

ENGS = ("pe", "act", "dve", "pool", "sp")

SEQ = 2048
DM = 1024
NT = 16
DFF = 2816
NL = 2
EPS = 1e-6
NCORES = 8


class Tok:
    __slots__ = ("name", "w", "r")

    def __init__(self, name=""):
        self.name = name
        self.w = None
        self.r = []


class Op:
    __slots__ = ("eng", "fn", "deps", "signal", "seq", "is_dma", "dsem", "dval")

    def __init__(self, eng, fn, is_dma=False):
        self.eng = eng
        self.fn = fn
        self.deps = []
        self.signal = False
        self.seq = None
        self.is_dma = is_dma
        self.dsem = None
        self.dval = None


class Sched:
    def __init__(self, nc, n_dma_sems=8):
        self.nc = nc
        self.q = {e: [] for e in ENGS}
        self.n_dma_sems = n_dma_sems

    def add(self, eng, fn, reads=(), writes=(), is_dma=False):
        op = Op(eng, fn, is_dma)
        deps = []
        for t in reads:
            if t.w is not None:
                deps.append((t.w, "raw"))
        for t in writes:
            if t.w is not None:
                deps.append((t.w, "waw"))
            for r in t.r:
                deps.append((r, "war"))
        seen = set()
        for d, kind in deps:
            if d is op or id(d) in seen:
                continue
            if d.eng == eng and not d.is_dma and not is_dma:
                if eng in ("pe", "sp"):
                    continue
                if kind != "raw":
                    continue
            seen.add(id(d))
            op.deps.append(d)
            d.signal = True
        for t in reads:
            t.r.append(op)
        for t in writes:
            t.w = op
            t.r = []
        if is_dma:
            op.signal = True
        self.q[eng].append(op)
        return op

    def barrier(self):
        lasts = []
        for e in ENGS:
            ql = self.q[e]
            for o in reversed(ql):
                if not o.is_dma and o.fn is not None:
                    lasts.append(o)
                    break
            cnt = 0
            for o in reversed(ql):
                if o.is_dma:
                    lasts.append(o)
                    cnt += 1
                    if cnt >= self.n_dma_sems:
                        break
        for e in ENGS:
            op = Op(e, None)
            for d in lasts:
                if d.eng == e and not d.is_dma:
                    continue
                op.deps.append(d)
                d.signal = True
            self.q[e].append(op)

    def emit(self, sems, dma_sems):
        nc = self.nc
        for e in ENGS:
            c = 0
            k = 0
            for o in self.q[e]:
                if o.is_dma:
                    o.dsem = dma_sems[e][k % self.n_dma_sems]
                    o.dval = 16 * (k // self.n_dma_sems + 1)
                    k += 1
                elif o.signal:
                    c += 1
                    o.seq = c
        handles = {"pe": "tensor", "act": "scalar", "dve": "vector", "pool": "gpsimd", "sp": "sync"}
        stats = {}
        with nc.Block() as block:
            for e in ENGS:
                ops = self.q[e]
                if not ops:
                    continue

                def body(eng, e=e, ops=ops):
                    waited = {}
                    nw = 0
                    for o in ops:
                        wl = []
                        for d in o.deps:
                            if d.is_dma:
                                wl.append((d.dsem, d.dval))
                            else:
                                wl.append((sems[d.eng], d.seq))
                        if o.is_dma and o.dval > 16:
                            wl.append((o.dsem, o.dval - 16))
                        for sem, val in wl:
                            key = id(sem)
                            if waited.get(key, 0) >= val:
                                continue
                            waited[key] = val
                            eng.wait_ge(sem, val)
                            nw += 1
                        if o.fn is None:
                            continue
                        ins = o.fn(eng)
                        if o.is_dma:
                            ins.then_inc(o.dsem, 16)
                        elif o.signal:
                            ins.then_inc(sems[e], 1)
                    for o in ops[::-1]:
                        if o.is_dma:
                            key = id(o.dsem)
                            if waited.get(key, 0) >= o.dval:
                                continue
                            waited[key] = o.dval
                            eng.wait_ge(o.dsem, o.dval)
                    stats[e] = (len(ops), nw)

                getattr(block, handles[e])(body)
        return stats


A0, B0, QI0, KI0, WI0, C0, FG0, D0 = 0, 768, 1536, 1792, 1824, 1832, 2600, 2604


def _swap(cols, d):
    c = cols.reshape(-1, d)
    h = d // 2
    return np.concatenate([c[:, h:], c[:, :h]], axis=1).reshape(-1)


def _inter(c, d):
    s = _swap(c, d)
    return np.concatenate([c[0:128], s[0:128], c[128:256], s[128:256]])


def _win_units():
    ar = np.arange
    u = {}
    u["A_f0"] = A0 + ar(512)
    u["A_t"] = A0 + 512 + ar(256)
    u["C_f0"] = C0 + ar(512)
    u["C_t"] = np.concatenate([C0 + 512 + ar(256), FG0 + ar(4)])
    u["D_f0"] = _inter(D0 + ar(256), 32)
    u["D_f1"] = _inter(D0 + 256 + ar(256), 32)
    u["D_t"] = D0 + 512 + ar(256)
    u["B_f0"] = _inter(B0 + ar(256), 64)
    u["B_f1"] = _inter(B0 + 256 + ar(256), 64)
    u["B_f2"] = _inter(QI0 + ar(256), 32)
    ki4 = np.tile(KI0 + ar(32), 4)
    u["B_f3"] = np.concatenate([ki4, _swap(ki4, 32)])
    u["B_t"] = np.concatenate([B0 + 512 + ar(256), WI0 + ar(8)])
    offs = {}
    o = 0
    cols = []
    for k, v in u.items():
        offs[k] = (o, len(v))
        o += len(v)
        cols.append(v)
    return offs, np.concatenate(cols)


WIN_OFFS, WIN_COLS = _win_units()
NCOL = len(WIN_COLS)


def _rope_tables(d):
    half = d // 2
    inv = (10000.0 ** (-np.arange(0, d, 2, dtype=np.float32) / np.float32(d))).astype(np.float32)
    pos = np.arange(SEQ, dtype=np.float32)
    ang = (pos[:, None] * inv[None, :]).astype(np.float32)
    cos = np.cos(ang).astype(np.float32).T
    sin = np.sin(ang).astype(np.float32).T
    p = np.arange(128)
    j = p % half
    sign = np.where((p % d) < half, -1.0, 1.0).astype(np.float32)
    cs = np.ascontiguousarray(cos[j])
    ss = np.ascontiguousarray(sin[j] * sign[:, None])
    return cs.astype(np.float32), ss.astype(np.float32)


def build_program(n_layers=NL, mixers="ACDB", do_ffn=True, do_final=True, debug=False, nbis=24):
    nc = bass.Bass("TRN2", target_bir_lowering=False)
    dram = lambda name, shape, dt=F32, kind="ExternalInput": nc.dram_tensor(name, list(shape), dt, kind=kind).ap()
    x_d = dram("x", [SEQ, DM])
    win_d = dram("win", [NL, DM, NCOL])
    wo_d = dram("wo", [NL, DM, DM])
    wg_d = dram("wg", [NL, DM, DFF])
    wu_d = dram("wu", [NL, DM, DFF])
    wd_d = dram("wd", [NL, DFF, DM])
    g1_d = dram("g1", [NL, DM])
    g2_d = dram("g2", [NL, DM])
    gf_d = dram("gf", [1, DM])
    relb_d = dram("relb", [NL, 128, 4 * 2 * 128])
    relc_d = dram("relc", [NL, 4])
    fb_d = dram("fb", [NL, 4])
    lam_d = dram("lamv", [NL, 128])
    dng_d = dram("dng", [NL, 64])
    cs64_d = dram("cs64", [128, SEQ])
    ss64_d = dram("ss64", [128, SEQ])
    cs32_d = dram("cs32", [128, SEQ])
    ss32_d = dram("ss32", [128, SEQ])
    out_d = dram("out", [SEQ, DM], F32, "ExternalOutput")
    xres_d = dram("xres", [SEQ, DM], F32, "Internal")
    if debug:
        dbg_h = dram("dbg_h", [128, 8 * SEQ], BF16, "ExternalOutput")
        dbg_m = dram("dbg_m", [128, 8 * SEQ], BF16, "ExternalOutput")
        dbg_x = dram("dbg_x", [SEQ, DM], F32, "ExternalOutput")

    ARENA = 211968
    with ExitStack() as es:
        arena = es.enter_context(nc.sbuf_tensor("arena", [128, ARENA // 4], F32))
        psb = [es.enter_context(nc.psum_tensor(f"ps{i}", [128, 512], F32)) for i in range(8)]
        sems = {e: es.enter_context(nc.semaphore("s_" + e)) for e in ENGS}
        dsems = {e: [es.enter_context(nc.semaphore(f"d_{e}{i}")) for i in range(8)] for e in ENGS}
        S = Sched(nc)

        def carve(off, shape, dt):
            esz = 4 if dt == F32 else 2
            n = int(np.prod(shape[1:]))
            assert off % 4 == 0 and (n * esz) % 4 == 0, (off, shape)
            a = arena[0:shape[0], off // 4: off // 4 + n * esz // 4]
            if dt != F32:
                a = a.bitcast(dt)
            if len(shape) == 3:
                a = a.rearrange("p (a b) -> p a b", a=shape[1])
            elif len(shape) == 4:
                a = a.rearrange("p (a b c) -> p a b c", a=shape[1], b=shape[2])
            return a

        hT = carve(0, [128, 8, SEQ], BF16)
        mixT = carve(32768, [128, 8, SEQ], BF16)
        U = 65536
        X = carve(U, [128, NT, DM], F32)
        QT = carve(U + 0, [128, 2, SEQ], BF16)
        KT = carve(U + 8192, [128, 2, SEQ], BF16)
        V = carve(U + 16384, [128, NT, 4, 128], BF16)
        qiT = carve(U + 32768, [128, 2, SEQ], BF16)
        kiT = carve(U + 40960, [128, SEQ], BF16)
        wiT = carve(U + 45056, [128, NT, 8], F32)
        fgT = carve(U + 45568, [128, NT, 4], F32)
        cumT = carve(U + 45824, [128, NT, 4], F32)
        cbT = carve(U + 46080, [128, NT, 4], F32)
        gexT = carve(U + 46336, [128, NT, 4], F32)
        biasC = carve(U + 46592, [128, 4, 136], F32)
        W = 131072
        wunit = [carve(W + i * 8192, [128, 8, 512], BF16) for i in range(4)]
        hb = [carve(W + 32768 + i * 2048, [128, DM], BF16) for i in range(2)]
        junk = carve(W + 36864, [128, DM], BF16)
        PT = [carve(W + 38912 + i * 1024, [128, 512], BF16) for i in range(4)] + \
             [carve(U + 62592 + i * 1024, [128, 512], BF16) for i in range(2)]
        tmp = [carve(W + 43008 + i * 2048, [128, 512], F32) for i in range(4)]
        obuf = [carve(W + 43008 + i * 4096, [128, DM], F32) for i in range(2)]
        G = carve(W + 51200, [128, DM], F32)
        ident = carve(W + 55296, [128, 128], BF16)
        cst = carve(W + 55296 + 256, [128, 192], F32)
        relbT = carve(W + 56320, [128, 4, 2, 128], F32)
        TAB = [carve(W + 60416 + i * 8192, [128, SEQ], F32) for i in range(2)]
        small2 = carve(W + 76800, [128, 1024], F32)
        assert W + 80896 == ARENA
        scoreb = carve(0, [128, SEQ], F32)
        selb = carve(8192, [128, SEQ], BF16)
        selT = carve(12288, [128, NT, 512], BF16)
        junkb = carve(28672, [128, SEQ], BF16)

        eps_t = cst[:, 0:1]
        relc_t = cst[:, 4:8]
        fb_t = cst[:, 8:12]
        lam_t = cst[:, 12:13]
        nlam_t = cst[:, 13:14]
        lamw = cst[:, 16:48]
        dngT = cst[:, 64:128]
        dngc2 = cst[:, 52:53]
        lamv = small2[:, 0:128]
        tri = small2[:, 128:256]
        mhalf = small2[:, 256:384]
        ones_f = small2[:, 384:512]
        bis = small2[:, 512:640]
        small = small2[:, 640:896]
        identf = tmp[0][:, 0:128]

        T_ps = [Tok(f"ps{i}") for i in range(8)]
        T_hT = [Tok(f"hT{i}") for i in range(NT)]
        T_mix = [[Tok(f"mix{m}{g}") for g in range(4)] for m in range(4)]
        T_X = [Tok(f"X{i}") for i in range(NT)]
        T_QT = [[Tok() for _ in range(4)] for _ in range(2)]
        T_KT = [[Tok() for _ in range(4)] for _ in range(2)]
        T_qi = [[Tok() for _ in range(4)] for _ in range(2)]
        T_ki = [Tok() for _ in range(4)]
        T_V = [Tok() for _ in range(NT)]
        T_wu = [Tok() for _ in range(4)]
        T_hb = [Tok() for _ in range(2)]
        T_PT = [Tok() for _ in range(6)]
        T_tmp = [Tok() for _ in range(4)]
        T_otm = [Tok() for _ in range(2)]
        T_dtmp = Tok()
        T_small = Tok()
        T_G = Tok()
        T_cst = Tok()
        T_tab = Tok()
        T_misc = Tok()
        T_junk = Tok()
        T_ss = [Tok() for _ in range(NT)]
        T_relb = Tok()
        T_score = Tok()
        T_sel = Tok()
        T_selT = [Tok() for _ in range(4)]
        T_bis = Tok()
        T_xres = Tok()
        T_out = Tok()
        T_dbg = Tok()

        def mm(out, lhsT, rhs, start, stop, reads, writes, **kw):
            S.add("pe", lambda e: e.matmul(out, lhsT=lhsT, rhs=rhs, start=start, stop=stop, **kw), reads, writes)

        def tr(out, in_, reads, writes):
            S.add("pe", lambda e: e.transpose(out=out, in_=in_, identity=ident), reads, writes)

        def act(out, in_, func, reads, writes, **kw):
            S.add("act", lambda e: e.activation(out=out, in_=in_, func=func, **kw), reads, writes)

        def ts(eng, out, in0, s1, s2, op0, op1, reads, writes, **kw):
            if op1 is None:
                S.add(eng, lambda e: e.tensor_scalar(out=out, in0=in0, scalar1=s1, scalar2=None, op0=op0, **kw), reads, writes)
            else:
                S.add(eng, lambda e: e.tensor_scalar(out=out, in0=in0, scalar1=s1, scalar2=s2, op0=op0, op1=op1, **kw), reads, writes)

        def tt(eng, out, in0, in1, op, reads, writes):
            S.add(eng, lambda e: e.tensor_tensor(out=out, in0=in0, in1=in1, op=op), reads, writes)

        def stt(out, in0, scalar, in1, op0, op1, reads, writes):
            S.add("dve", lambda e: e.scalar_tensor_tensor(out=out, in0=in0, scalar=scalar, in1=in1, op0=op0, op1=op1), reads, writes)

        def cp(eng, out, in_, reads, writes):
            if eng == "act":
                act(out, in_, AF.Copy, reads, writes)
            else:
                S.add(eng, lambda e: e.tensor_copy(out=out, in_=in_), reads, writes)

        def memset(eng, ap, val, reads, writes):
            S.add(eng, lambda e: e.memset(ap, val), reads, writes)

        def dma(eng, out, in_, reads, writes):
            S.add(eng, lambda e: e.dma_start(out=out, in_=in_), reads, writes, is_dma=True)

        def bcast_rows(src_row_ap, nparts=128):
            return src_row_ap.broadcast_to([nparts, src_row_ap.shape[-1]])

        ps_rr = [0]

        def next_ps():
            i = ps_rr[0] % 8
            ps_rr[0] += 1
            return i

        wlist = []

        def wsrc(d_ap, l, rows0, nrows, c0, ncols):
            return (d_ap[l, rows0:rows0 + nrows, c0:c0 + ncols], nrows // 128, ncols)

        order = [m for m in "ACDB" if m in mixers]
        for l in range(n_layers):
            for m in order:
                names = {"A": ["A_f0", "A_t"], "C": ["C_f0", "C_t"], "D": ["D_f0", "D_f1", "D_t"],
                         "B": ["B_f0", "B_f1", "B_f2", "B_f3", "B_t"]}[m]
                for nm in names:
                    o, n = WIN_OFFS[nm]
                    wlist.append(wsrc(win_d, l, 0, DM, o, n))
            for hf in range(2):
                wlist.append(wsrc(wo_d, l, 0, DM, hf * 512, 512))
            if do_ffn:
                for (f0, nf) in ((0, 1024), (1024, 1024), (2048, 768)):
                    for c0 in range(f0, f0 + nf, 512):
                        nn = min(512, f0 + nf - c0)
                        wlist.append(wsrc(wg_d, l, 0, DM, c0, nn))
                        wlist.append(wsrc(wu_d, l, 0, DM, c0, nn))
                    for hf in range(2):
                        wlist.append(wsrc(wd_d, l, f0, nf, hf * 512, 512))
        wstate = {"next_load": 0, "next_use": 0}

        def w_issue():
            i = wstate["next_load"]
            if i >= len(wlist):
                return
            src, kc, ncols = wlist[i]
            slot = i % 4
            dma("pool", wunit[slot][:, 0:kc, 0:ncols], src.rearrange("(kc p) c -> p kc c", p=128), [], [T_wu[slot]])
            wstate["next_load"] += 1

        def w_get():
            i = wstate["next_use"]
            wstate["next_use"] += 1
            assert i < wstate["next_load"]
            return i % 4

        def w_release():
            w_issue()

        memset("pool", identf, 1.0, [], [T_tmp[0]])
        S.add("pool", lambda e: e.affine_select(out=identf, in_=identf, pattern=[[1, 128]], compare_op=ALU.is_equal,
                                                fill=0.0, base=0, channel_multiplier=-1), [T_tmp[0]], [T_tmp[0]])
        cp("dve", ident, identf, [T_tmp[0]], [T_cst])
        memset("dve", eps_t, EPS, [], [T_cst])
        memset("pool", tri, 1.0, [], [T_misc])
        S.add("pool", lambda e: e.affine_select(out=tri, in_=tri, pattern=[[1, 128]], compare_op=ALU.is_ge,
                                                fill=0.0, base=0, channel_multiplier=-1), [T_misc], [T_misc])
        memset("pool", mhalf, 1.0, [], [T_misc])
        S.add("pool", lambda e: e.affine_select(out=mhalf, in_=mhalf, pattern=[[0, 128]], compare_op=ALU.is_ge,
                                                fill=0.0, base=64, channel_multiplier=-1), [T_misc], [T_misc])
        memset("pool", ones_f, 1.0, [], [T_misc])
        for _ in range(4):
            w_issue()

        def norm_to_hT(l, gsrc):
            dma("sp", G, bcast_rows(gsrc), [], [T_G])
            for tb in range(NT):
                b = tb % 2
                ssq = small[:, tb:tb + 1]
                std = small[:, 16 + tb:17 + tb]
                rstd = small[:, 32 + tb:33 + tb]
                act(junk, X[:, tb, :], AF.Square, [T_X[tb]], [T_junk, T_ss[tb]], accum_out=ssq)
                act(std, ssq, AF.Sqrt, [T_ss[tb], T_cst], [T_ss[tb]], scale=1.0 / DM, bias=eps_t)
                S.add("dve", lambda e, o=rstd, i=std: e.reciprocal(out=o, in_=i), [T_ss[tb]], [T_ss[tb]])
                stt(hb[b], X[:, tb, :], rstd, G, ALU.mult, ALU.mult, [T_X[tb], T_ss[tb], T_G], [T_hb[b]])
                pi = next_ps()
                pT = psb[pi][:, :].bitcast(BF16)
                for kc in range(8):
                    tr(pT[:, kc * 128:(kc + 1) * 128], hb[b][:, kc * 128:(kc + 1) * 128], [T_hb[b], T_cst], [T_ps[pi]])
                cp("act", hT[:, :, tb * 128:(tb + 1) * 128], pT.rearrange("p (a b) -> p a b", a=8), [T_ps[pi]], [T_hT[tb]])

        def proj_feature(slot, nchunks, evac):
            for ci in range(nchunks):
                for tg in range(4):
                    pi = next_ps()
                    for kc in range(8):
                        mm(psb[pi][:, :], wunit[slot][:, kc, ci * 128:(ci + 1) * 128], hT[:, kc, tg * 512:(tg + 1) * 512],
                           kc == 0, kc == 7, [T_wu[slot]] + T_hT[tg * 4:tg * 4 + 4], [T_ps[pi]])
                    evac(ci, tg, pi)

        def proj_feature_pairs(slot, npairs, evac):
            for pj in range(npairs):
                for tg in range(4):
                    pis = []
                    for s in range(2):
                        ci = pj * 2 + s
                        pi = next_ps()
                        pis.append(pi)
                        for kc in range(8):
                            mm(psb[pi][:, :], wunit[slot][:, kc, ci * 128:(ci + 1) * 128], hT[:, kc, tg * 512:(tg + 1) * 512],
                               kc == 0, kc == 7, [T_wu[slot]] + T_hT[tg * 4:tg * 4 + 4], [T_ps[pi]])
                    evac(pj, tg, pis)

        def proj_token(slot, ncols, evac):
            for tb in range(NT):
                pi = next_ps()
                for kc in range(8):
                    mm(psb[pi][:, 0:ncols], hT[:, kc, tb * 128:(tb + 1) * 128], wunit[slot][:, kc, 0:ncols],
                       kc == 0, kc == 7, [T_wu[slot], T_hT[tb]], [T_ps[pi]])
                evac(tb, pi)

        ev_rr = [0]

        def plain_evac(dst_fn, tok_fn):
            def f(ci, tg, pi):
                eng = "act" if ev_rr[0] % 2 == 0 else "dve"
                ev_rr[0] += 1
                cp(eng, dst_fn(ci, tg), psb[pi][:, :], [T_ps[pi]], [tok_fn(ci, tg)])
            return f

        rope_rr = [0]

        def rope_evac(dst_fn, tok_fn):
            def f(pj, tg, pis):
                k = rope_rr[0] % 2
                rope_rr[0] += 1
                t1, t2 = tmp[2 * k], tmp[2 * k + 1]
                tt("dve", t1, psb[pis[0]][:, :], TAB[0][:, tg * 512:(tg + 1) * 512], ALU.mult, [T_ps[pis[0]], T_tab], [T_tmp[2 * k]])
                tt("dve", t2, psb[pis[1]][:, :], TAB[1][:, tg * 512:(tg + 1) * 512], ALU.mult, [T_ps[pis[1]], T_tab], [T_tmp[2 * k + 1]])
                tt("pool", dst_fn(pj, tg), t1, t2, ALU.add, [T_tmp[2 * k], T_tmp[2 * k + 1]], [tok_fn(pj, tg)])
            return f

        def v_evac(extra=None):
            def f(tb, pi):
                eng = "act" if tb % 2 == 0 else "dve"
                cp(eng, V[:, tb, :, 0:64], psb[pi][:, 0:256].rearrange("p (h d) -> p h d", h=4), [T_ps[pi]], [T_V[tb]])
                if extra is not None:
                    extra(tb, pi)
            return f

        def load_tables(d):
            a, b = (cs64_d, ss64_d) if d == 64 else (cs32_d, ss32_d)
            dma("sp", TAB[0], a, [], [T_tab])
            dma("sp", TAB[1], b, [], [T_tab])

        st_rr = [0]
        op_rr = [0]
        pt_rr = [0]
        ST_BANKS = [0, 1, 2]
        OP_BANKS = [3, 4, 5, 6]
        TR_BANK = 7

        pend = []
        SKEW = 2

        def pipe_push(fn, skew=SKEW):
            pend.append([fn, []])
            while len(pend) > skew:
                f, cbs = pend.pop(0)
                f()
                for c in cbs:
                    c()

        def pipe_cb(fn):
            if pend:
                pend[-1][1].append(fn)
            else:
                fn()

        def pipe_flush():
            while pend:
                f, cbs = pend.pop(0)
                f()
                for c in cbs:
                    c()

        st_banks = {"l": [0, 1, 2]}

        def attn_group(m, g, kbs, vheads):
            obs = []
            for _ in vheads:
                obs.append(OP_BANKS[op_rr[0] % 4])
                op_rr[0] += 1
            SKEW_ = len(vheads) * (2 if len(vheads) <= 2 else 1)
            first = True
            for kb in kbs:
                if m == "A":
                    qbs = [qb for qb in range(4 * g, 4 * g + 4) if kb <= qb <= kb + 4]
                else:
                    qbs = [qb for qb in range(4 * g, 4 * g + 4) if qb >= kb]
                if not qbs:
                    continue
                n = len(qbs) * 128
                q0 = qbs[0] * 128
                for vi, vhd in enumerate(vheads):
                    stl = st_banks["l"]
                    sb_i = stl[st_rr[0] % len(stl)]
                    st_rr[0] += 1
                    pt_i = pt_rr[0] % 6
                    pt_rr[0] += 1
                    base, rows, tile = vhd["base"], vhd["rows"], vhd["tile"]
                    lhsT = KT[base:base + rows, tile, kb * 128:(kb + 1) * 128]
                    rhs = QT[base:base + rows, tile, q0:q0 + n]
                    kw = {}
                    if base == 96:
                        kw["tile_position"] = (96, 0)
                    mm(psb[sb_i][:, 0:n], lhsT, rhs, True, True, [vhd["qt"][g], vhd["kt"][kb // 4]], [T_ps[sb_i]], **kw)
                    vhd["exp"](kb, qbs, sb_i, pt_i)
                    vhd["post"](kb, qbs, pt_i)

                    def pv(kb=kb, qbs=qbs, pt_i=pt_i, first=first, n=n, ob=obs[vi], vh=vhd["vh"]):
                        c0 = (qbs[0] - 4 * g) * 128
                        mm(psb[ob][:, c0:c0 + n], V[:, kb, vh, :], PT[pt_i][:, 0:n],
                           first, False, [T_PT[pt_i], T_V[kb]], [T_ps[ob]], skip_group_check=True)
                    pipe_push(pv, SKEW_)
                    bg_tick()
                first = False
            return obs

        NB = TR_BANK
        bg = {"chunks": [], "per": 0}

        def bg_tick():
            for _ in range(bg["per"]):
                if bg["chunks"]:
                    bg["chunks"].pop(0)()

        def bg_drain():
            while bg["chunks"]:
                bg["chunks"].pop(0)()
            bg["per"] = 0

        def norm_fm(ob, mi, h, g):
            k = rope_rr[0] % 4
            rope_rr[0] += 1
            act(tmp[k][0:64, :], psb[ob][64:128, :], AF.Ln, [T_ps[ob]], [T_tmp[k]])
            act(tmp[k][0:64, :], tmp[k][0:64, :], AF.Exp, [T_tmp[k]], [T_tmp[k]], scale=-1.0)
            r0 = (h % 2) * 64
            tt("dve", mixT[r0:r0 + 64, 2 * mi + h // 2, g * 512:(g + 1) * 512], psb[ob][0:64, :], tmp[k][0:64, :], ALU.mult,
               [T_ps[ob], T_tmp[k]], [T_mix[mi][g]])

        T_tmpX = [[Tok() for _ in range(4)] for _ in range(2)]

        def d_fork():
            for k in range(4):
                memset("pool", tmp[k][0:1, 0:1], 0.0, [T_tmp[k]], [T_tmp[k], T_tmpX[0][k], T_tmpX[1][k]])

        def d_join():
            for k in range(4):
                memset("pool", tmp[k][0:1, 0:1], 0.0, [T_tmpX[0][k], T_tmpX[1][k]], [T_tmp[k]])

        def d_combine_fm(obs, mi, h, g):
            st_ = h % 2
            p0 = st_ * 64
            tk = T_tmpX[st_]
            t0, t1, t2, t3 = [t[p0:p0 + 64, :] for t in tmp]
            o1 = psb[obs[0]]
            o2 = psb[obs[1]]
            act(t0, o1[64:128, :], AF.Ln, [T_ps[obs[0]]], [tk[0]])
            act(t0, t0, AF.Exp, [tk[0]], [tk[0]], scale=-1.0)
            act(t1, o2[64:128, :], AF.Ln, [T_ps[obs[1]]], [tk[1]])
            act(t1, t1, AF.Exp, [tk[1]], [tk[1]], scale=-1.0)
            tt("dve", t2, o1[0:64, :], t0, ALU.mult, [T_ps[obs[0]], tk[0]], [tk[2]])
            tt("dve", t3, o2[0:64, :], t1, ALU.mult, [T_ps[obs[1]], tk[1]], [tk[3]])
            stt(t2, t3, nlam_t[p0:p0 + 64, :], t2, ALU.mult, ALU.add, [tk[3], tk[2], T_cst], [tk[2]])
            tt("pool", t3, t2, t2, ALU.mult, [tk[2]], [tk[3]])
            mm(psb[NB][0:64, :], ones_f[p0:p0 + 64, 0:64], t3, True, True, [tk[3], T_misc], [T_ps[NB]])
            act(t0, psb[NB][0:64, :], AF.Ln, [T_ps[NB], T_cst], [tk[0]], scale=1.0 / 64, bias=eps_t[p0:p0 + 64, :])
            act(t1, t0, AF.Exp, [tk[0]], [tk[1]], scale=-0.5)
            r0 = (h % 2) * 64
            stt(mixT[r0:r0 + 64, 2 * mi + h // 2, g * 512:(g + 1) * 512], t2, dngc2[p0:p0 + 64, :], t1, ALU.mult, ALU.mult,
                [tk[2], tk[1], T_cst], [T_mix[mi][g]])

        otm_rr = [0]

        for l in range(n_layers):
            lam_init = 0.8 - 0.6 * math.exp(-0.3 * l)
            if l == 0:
                for tb in range(NT):
                    dma("sp", X[:, tb, :], x_d[tb * 128:(tb + 1) * 128, :], [], [T_X[tb]])
            dma("sp", relc_t, bcast_rows(relc_d[l:l + 1, :]), [], [T_cst])
            dma("sp", fb_t, bcast_rows(fb_d[l:l + 1, :]), [], [T_cst])
            dma("sp", lamv, bcast_rows(lam_d[l:l + 1, :]), [], [T_misc])
            dma("sp", dngT, bcast_rows(dng_d[l:l + 1, :]), [], [T_cst])
            dma("sp", relbT, relb_d[l].rearrange("k (h d q) -> k h d q", h=4, d=2), [], [T_relb])
            tt("dve", lamw, lamv[:, 0:32], lamv[:, 32:64], ALU.mult, [T_misc], [T_cst])
            S.add("dve", lambda e: e.tensor_reduce(out=cst[:, 48:49], in_=lamw, axis=AX.X, op=ALU.add), [T_cst], [T_cst])
            tt("dve", lamw, lamv[:, 64:96], lamv[:, 96:128], ALU.mult, [T_misc, T_cst], [T_cst])
            S.add("dve", lambda e: e.tensor_reduce(out=cst[:, 49:50], in_=lamw, axis=AX.X, op=ALU.add), [T_cst], [T_cst])
            act(cst[:, 50:52], cst[:, 48:50], AF.Exp, [T_cst], [T_cst])
            tt("dve", lam_t, cst[:, 50:51], cst[:, 51:52], ALU.subtract, [T_cst], [T_cst])
            ts("dve", lam_t, lam_t, lam_init, None, ALU.add, None, [T_cst], [T_cst])
            ts("dve", nlam_t, lam_t, -1.0, None, ALU.mult, None, [T_cst], [T_cst])
            ts("dve", dngT, dngT, 1.0 - lam_init, None, ALU.mult, None, [T_cst], [T_cst])
            dma("sp", dngc2[0:64, :], dng_d[l].rearrange("(d o) -> d o", o=1), [], [T_cst])
            dma("sp", dngc2[64:128, :], dng_d[l].rearrange("(d o) -> d o", o=1), [], [T_cst])
            ts("dve", dngc2, dngc2, 1.0 - lam_init, None, ALU.mult, None, [T_cst], [T_cst])

            norm_to_hT(l, g1_d[l:l + 1, :])
            if l > 0:
                for tb in range(NT):
                    dma("sp", xres_d[tb * 128:(tb + 1) * 128, :], X[:, tb, :], [T_X[tb]], [T_xres])
            if debug and l == 0:
                dma("sp", dbg_h, hT.rearrange("p a b -> p (a b)"), T_hT, [T_dbg])
            S.barrier()
            memset("pool", V[:, :, :, 64:128], 1.0, [], T_V)

            for mi_, m in enumerate(order):
                mi = "ABCD".index(m)
                if m in "AC":
                    s0 = w_get()
                    proj_feature(s0, 4, plain_evac(
                        lambda ci, tg: (QT if ci < 2 else KT)[:, ci % 2, tg * 512:(tg + 1) * 512],
                        lambda ci, tg: (T_QT if ci < 2 else T_KT)[ci % 2][tg]))
                    w_release()
                    s1 = w_get()
                    if m == "A":
                        proj_token(s1, 256, v_evac())
                    else:
                        def fg_extra(tb, pi):
                            cp("dve", fgT[:, tb, :], psb[pi][:, 256:260], [T_ps[pi]], [T_misc])
                        proj_token(s1, 260, v_evac(fg_extra))
                    w_release()
                elif m == "D":
                    load_tables(32)
                    for (dst, dtk) in ((QT, T_QT), (KT, T_KT)):
                        s0 = w_get()
                        proj_feature_pairs(s0, 2, rope_evac(
                            lambda pj, tg, dst=dst: dst[:, pj, tg * 512:(tg + 1) * 512],
                            lambda pj, tg, dtk=dtk: dtk[pj][tg]))
                        w_release()
                    s1 = w_get()
                    proj_token(s1, 256, v_evac())
                    w_release()
                elif m == "B":
                    load_tables(64)
                    for (dst, dtk) in ((QT, T_QT), (KT, T_KT)):
                        s0 = w_get()
                        proj_feature_pairs(s0, 2, rope_evac(
                            lambda pj, tg, dst=dst: dst[:, pj, tg * 512:(tg + 1) * 512],
                            lambda pj, tg, dtk=dtk: dtk[pj][tg]))
                        w_release()
                    load_tables(32)
                    s0 = w_get()
                    proj_feature_pairs(s0, 2, rope_evac(
                        lambda pj, tg: qiT[:, pj, tg * 512:(tg + 1) * 512],
                        lambda pj, tg: T_qi[pj][tg]))
                    w_release()
                    s0 = w_get()
                    proj_feature_pairs(s0, 1, rope_evac(
                        lambda pj, tg: kiT[:, tg * 512:(tg + 1) * 512],
                        lambda pj, tg: T_ki[tg]))
                    w_release()
                    s1 = w_get()

                    def wi_extra(tb, pi):
                        cp("dve", wiT[:, tb, :], psb[pi][:, 256:264], [T_ps[pi]], [T_misc])
                    proj_token(s1, 264, v_evac(wi_extra))
                    w_release()

                if m == "A":
                    def a_exp(h):
                        def f(kb, qbs, sb_i, pt_i):
                            far = [qb for qb in qbs if qb - kb >= 2]
                            for j, qb in enumerate(qbs):
                                dl = qb - kb
                                if dl <= 1:
                                    k = rope_rr[0] % 4
                                    rope_rr[0] += 1
                                    stt(tmp[k][:, 0:128], psb[sb_i][:, j * 128:(j + 1) * 128], 0.125, relbT[:, h, dl, :],
                                        ALU.mult, ALU.add, [T_ps[sb_i], T_relb], [T_tmp[k]])
                                    act(PT[pt_i][:, j * 128:(j + 1) * 128], tmp[k][:, 0:128], AF.Exp, [T_tmp[k]], [T_PT[pt_i]])
                            if far:
                                j0 = qbs.index(far[0])
                                act(PT[pt_i][:, j0 * 128:(j0 + len(far)) * 128], psb[sb_i][:, j0 * 128:(j0 + len(far)) * 128], AF.Exp,
                                    [T_ps[sb_i], T_cst], [T_PT[pt_i]], scale=0.125, bias=relc_t[:, h:h + 1])
                        return f

                    def a_post(kb, qbs, pt_i):
                        for j, qb in enumerate(qbs):
                            if qb == kb:
                                memset("pool", PT[pt_i][64:128, j * 128:j * 128 + 64], 0.0, [T_PT[pt_i]], [T_PT[pt_i]])
                            if qb == kb + 4:
                                memset("pool", PT[pt_i][0:64, j * 128 + 64:j * 128 + 128], 0.0, [T_PT[pt_i]], [T_PT[pt_i]])

                    st_banks["l"] = [0, 1, 2, 7]
                    for g in range(4):
                        kbs = list(range(max(0, 4 * g - 4), 4 * g + 4))
                        for hp in range(2):
                            vhs = [dict(tile=hp, base=(h % 2) * 64, rows=64, vh=h, exp=a_exp(h), post=a_post, qt=T_QT[hp], kt=T_KT[hp])
                                   for h in (2 * hp, 2 * hp + 1)]
                            obs = attn_group("A", g, kbs, vhs)
                            for ob, h in zip(obs, (2 * hp, 2 * hp + 1)):
                                pipe_cb(lambda ob=ob, h=h, g=g: norm_fm(ob, mi, h, g))
                    pipe_flush()
                    st_banks["l"] = [0, 1, 2]

                elif m == "C":
                    fg2 = fgT.rearrange("p a b -> p (a b)")
                    tt("dve", fgT, fgT, fb_t.unsqueeze(1).to_broadcast([128, NT, 4]), ALU.add, [T_misc, T_cst], [T_misc])
                    act(fg2, fg2, AF.Exp, [T_misc], [T_misc], scale=-1.0)
                    act(fg2, fg2, AF.Ln, [T_misc], [T_misc], bias=1.0)
                    ts("dve", fg2, fg2, -1.0, None, ALU.mult, None, [T_misc], [T_misc])
                    memset("dve", gexT[:, 0, :], 0.0, [], [T_misc])
                    for tb in range(1, NT):
                        tt("dve", gexT[:, tb, :], gexT[:, tb - 1, :], fgT[:, tb - 1, :], ALU.add, [T_misc], [T_misc])
                    pi = next_ps()
                    pj = next_ps()
                    gex2 = gexT.rearrange("p a b -> p (a b)")
                    mm(psb[pi][:, 0:64], tri, fg2, True, False, [T_misc], [T_ps[pi]])
                    mm(psb[pi][:, 0:64], ones_f, gex2, False, True, [T_misc], [T_ps[pi]])
                    mm(psb[pj][:, 0:64], mhalf, fg2, True, False, [T_misc], [T_ps[pj]])
                    mm(psb[pj][:, 0:64], ones_f, gex2, False, True, [T_misc], [T_ps[pj]])
                    cp("dve", cumT.rearrange("p a b -> p (a b)"), psb[pi][:, 0:64], [T_ps[pi]], [T_misc])
                    cp("dve", cbT.rearrange("p a b -> p (a b)"), psb[pj][:, 0:64], [T_ps[pj]], [T_misc])
                    pair_idx = {}
                    pidx = 0
                    for qb in range(NT):
                        for h in range(4):
                            ts("dve", biasC[:, h, pidx:pidx + qb + 1], cumT[:, 0:qb + 1, h], -1.0, cbT[:, qb, h:h + 1],
                               ALU.mult, ALU.add, [T_misc], [T_misc])
                        for kb in range(qb + 1):
                            pair_idx[(qb, kb)] = pidx + kb
                        pidx += qb + 1

                    def c_exp(h):
                        def f(kb, qbs, sb_i, pt_i):
                            for j, qb in enumerate(qbs):
                                act(PT[pt_i][:, j * 128:(j + 1) * 128], psb[sb_i][:, j * 128:(j + 1) * 128], AF.Exp,
                                    [T_ps[sb_i], T_misc], [T_PT[pt_i]], scale=0.125,
                                    bias=biasC[:, h, pair_idx[(qb, kb)]:pair_idx[(qb, kb)] + 1])
                        return f

                    def c_post(kb, qbs, pt_i):
                        for j, qb in enumerate(qbs):
                            if qb == kb:
                                ap = PT[pt_i][:, j * 128:(j + 1) * 128]
                                S.add("pool", lambda e, ap=ap: e.affine_select(out=ap, in_=ap, pattern=[[1, 128]], compare_op=ALU.is_ge,
                                                                               fill=0.0, base=0, channel_multiplier=-1),
                                      [T_PT[pt_i]], [T_PT[pt_i]])

                    st_banks["l"] = [0, 1, 2, 7]
                    for g in range(4):
                        for hp in range(2):
                            vhs = [dict(tile=hp, base=(h % 2) * 64, rows=64, vh=h, exp=c_exp(h), post=c_post, qt=T_QT[hp], kt=T_KT[hp])
                                   for h in (2 * hp, 2 * hp + 1)]
                            obs = attn_group("C", g, list(range(4 * g + 4)), vhs)
                            for ob, h in zip(obs, (2 * hp, 2 * hp + 1)):
                                pipe_cb(lambda ob=ob, h=h, g=g: norm_fm(ob, mi, h, g))
                    pipe_flush()
                    st_banks["l"] = [0, 1, 2]

                elif m in "DB":
                    sc = 32 ** -0.5 if m == "D" else 0.125

                    def d_exp(kb, qbs, sb_i, pt_i):
                        n = len(qbs) * 128
                        act(PT[pt_i][:, 0:n], psb[sb_i][:, 0:n], AF.Exp, [T_ps[sb_i]], [T_PT[pt_i]], scale=sc)

                    def d_post(kb, qbs, pt_i):
                        for j, qb in enumerate(qbs):
                            if qb == kb:
                                memset("pool", PT[pt_i][64:128, j * 128:j * 128 + 64], 0.0, [T_PT[pt_i]], [T_PT[pt_i]])

                    if m == "D":
                        d_fork()
                        for g in range(4):
                            for h in range(4):
                                hp = h // 2
                                vhs = [dict(tile=hp, base=((h % 2) * 2 + r) * 32, rows=32, vh=h, exp=d_exp, post=d_post,
                                            qt=T_QT[hp], kt=T_KT[hp]) for r in range(2)]
                                obs = attn_group("D", g, list(range(4 * g + 4)), vhs)
                                pipe_cb(lambda obs=obs, h=h, g=g: d_combine_fm(obs, mi, h, g))
                        pipe_flush()
                        d_join()
                    else:
                        S.barrier()
                        KSEL = 256.0
                        selTs = [carve(0, [128, 12, 512], BF16), carve(12288, [128, 16, 512], BF16)]
                        junkb2 = carve(28672, [128, SEQ], BF16)
                        uleft = carve(U + 48768, [128, 3456], F32)
                        scb = [uleft[:, 1792:3456], uleft[:, 0:1792], TAB[1], TAB[0]]
                        selb2 = carve(W + 32768, [128, SEQ], BF16)
                        T_sc = [Tok() for _ in range(4)]
                        T_bq = [Tok() for _ in range(4)]
                        T_sT = [[Tok() for _ in range(4)] for _ in range(2)]
                        T_selb = Tok()

                        ajunk = G.bitcast(BF16)
                        ACT_CHAINS = (1, 3)

                        def b_select_chunks(g):
                            chunks = []
                            sT = selTs[g % 2]
                            tks = T_sT[g % 2]
                            active = []
                            for jq in range(4):
                                qb = 4 * g + jq
                                if qb < 2:
                                    chunks.append(lambda jq=jq, qb=qb: memset("pool", sT[:, 0:qb + 1, jq * 128:(jq + 1) * 128], 1.0, [], [tks[jq]]))
                                else:
                                    active.append((jq, qb))

                            def score_piece(jq, qb, gi, c0, n):
                                base = (gi % 4) * 32
                                kw = {"tile_position": (96, 0)} if base == 96 else {}
                                sb_i = ST_BANKS[st_rr[0] % 3]
                                st_rr[0] += 1
                                k = rope_rr[0] % 4
                                rope_rr[0] += 1
                                mm(psb[sb_i][:, 0:n], qiT[base:base + 32, gi // 4, qb * 128:(qb + 1) * 128],
                                   kiT[base:base + 32, c0:c0 + n], True, True,
                                   [T_qi[gi // 4][g]] + T_ki, [T_ps[sb_i]], **kw)
                                act(tmp[k][:, 0:n], psb[sb_i][:, 0:n], AF.Relu, [T_ps[sb_i]], [T_tmp[k]])
                                if gi == 0:
                                    ts("dve", scb[jq][:, c0:c0 + n], tmp[k][:, 0:n], wiT[:, qb, 0:1], None, ALU.mult, None,
                                       [T_tmp[k], T_misc], [T_sc[jq]])
                                else:
                                    stt(scb[jq][:, c0:c0 + n], tmp[k][:, 0:n], wiT[:, qb, gi:gi + 1], scb[jq][:, c0:c0 + n],
                                        ALU.mult, ALU.add, [T_tmp[k], T_misc, T_sc[jq]], [T_sc[jq]])

                            for jq, qb in active:
                                nk = (qb + 1) * 128
                                for gi in range(8):
                                    for c0 in range(0, nk, 512):
                                        n = min(512, nk - c0)
                                        chunks.append(lambda jq=jq, qb=qb, gi=gi, c0=c0, n=n: score_piece(jq, qb, gi, c0, n))
                                chunks.append(lambda jq=jq, qb=qb: memset("dve", scb[jq][0:64, qb * 128 + 64:qb * 128 + 128], -1.0e30,
                                                                          [T_sc[jq]], [T_sc[jq]]))

                            def init_piece():
                                for jq, qb in active:
                                    nk = (qb + 1) * 128
                                    memset("pool", bis[:, 4 * jq:4 * jq + 1], 0.0, [T_bq[jq]], [T_bq[jq]])
                                    if jq in ACT_CHAINS:
                                        memset("pool", bis[:, 4 * jq + 3:4 * jq + 4], float(nk) - 2.0 * KSEL + 0.5, [T_bq[jq]], [T_bq[jq]])
                            chunks.append(init_piece)

                            def round_piece(step):
                                for jq, qb in active:
                                    nk = (qb + 1) * 128
                                    c = 4 * jq
                                    if jq in ACT_CHAINS:
                                        act(ajunk[:, 0:nk], scb[jq][:, 0:nk], AF.Sign, [T_sc[jq], T_bq[jq]], [T_bq[jq]],
                                            bias=bis[:, c:c + 1], accum_out=bis[:, c + 1:c + 2])
                                    else:
                                        ts("dve", junkb2[:, 0:nk], scb[jq][:, 0:nk], bis[:, c:c + 1], None, ALU.is_ge, ALU.add,
                                           [T_sc[jq], T_bq[jq]], [T_bq[jq]], accum_out=bis[:, c + 1:c + 2])
                                for jq, qb in active:
                                    c = 4 * jq
                                    if jq in ACT_CHAINS:
                                        act(bis[:, c + 2:c + 3], bis[:, c + 1:c + 2], AF.Sign, [T_bq[jq]], [T_bq[jq]], bias=bis[:, c + 3:c + 4])
                                    else:
                                        ts("dve", bis[:, c + 2:c + 3], bis[:, c + 1:c + 2], KSEL, 2.0 * step,
                                           ALU.is_ge, ALU.mult, [T_bq[jq]], [T_bq[jq]])
                                for jq, qb in active:
                                    c = 4 * jq
                                    if jq in ACT_CHAINS:
                                        act(bis[:, c:c + 1], bis[:, c + 2:c + 3], AF.Identity, [T_bq[jq]], [T_bq[jq]],
                                            scale=-step, bias=bis[:, c:c + 1])
                                    else:
                                        stt(bis[:, c:c + 1], bis[:, c + 2:c + 3], -step, bis[:, c:c + 1],
                                            ALU.add, ALU.add, [T_bq[jq]], [T_bq[jq]])

                            step = 64.0
                            for it in range(nbis):
                                chunks.append(lambda step=step: round_piece(step))
                                step *= 0.5
                            fstep = step

                            def final_piece(jq, qb):
                                nk = (qb + 1) * 128
                                mid = bis[:, 4 * jq:4 * jq + 1]
                                if jq in ACT_CHAINS:
                                    ts("dve", mid, mid, -1.0, -2.0 * fstep, ALU.mult, ALU.add, [T_bq[jq]], [T_bq[jq]])
                                else:
                                    ts("dve", mid, mid, -2.0 * fstep, None, ALU.add, None, [T_bq[jq]], [T_bq[jq]])
                                ts("dve", selb2[:, 0:nk], scb[jq][:, 0:nk], mid, None, ALU.is_ge, None, [T_sc[jq], T_bq[jq]], [T_selb])
                                for k0 in range(0, qb + 1, 4):
                                    kn = min(4, qb + 1 - k0)
                                    pT = psb[TR_BANK][:, :].bitcast(BF16)
                                    for kk in range(kn):
                                        tr(pT[:, kk * 128:(kk + 1) * 128], selb2[:, (k0 + kk) * 128:(k0 + kk + 1) * 128],
                                           [T_selb, T_cst], [T_ps[TR_BANK]])
                                    cp("act", sT[:, k0:k0 + kn, jq * 128:(jq + 1) * 128],
                                       pT[:, 0:kn * 128].rearrange("p (a b) -> p a b", a=kn), [T_ps[TR_BANK]], [tks[jq]])
                            for jq, qb in active:
                                chunks.append(lambda jq=jq, qb=qb: final_piece(jq, qb))
                            return chunks

                        def b_attend(g):
                            sT = selTs[g % 2]
                            tks = T_sT[g % 2]

                            def b_post(kb, qbs, pt_i):
                                n = len(qbs) * 128
                                j0 = qbs[0] - 4 * g
                                for j, qb in enumerate(qbs):
                                    if qb == kb:
                                        memset("pool", PT[pt_i][64:128, j * 128:j * 128 + 64], 0.0, [T_PT[pt_i]], [T_PT[pt_i]])
                                tt("pool", PT[pt_i][:, 0:n], PT[pt_i][:, 0:n], sT[:, kb, j0 * 128:j0 * 128 + n], ALU.mult,
                                   [T_PT[pt_i]] + tks, [T_PT[pt_i]])

                            for hp in range(2):
                                vhs = [dict(tile=hp, base=(h % 2) * 64, rows=64, vh=h, exp=d_exp, post=b_post, qt=T_QT[hp], kt=T_KT[hp])
                                       for h in (2 * hp, 2 * hp + 1)]
                                obs = attn_group("B", g, list(range(4 * g + 4)), vhs)
                                for ob, h in zip(obs, (2 * hp, 2 * hp + 1)):
                                    pipe_cb(lambda ob=ob, h=h, g=g: norm_fm(ob, mi, h, g))
                            pipe_flush()

                        for c in b_select_chunks(0):
                            c()
                        for g in range(4):
                            if g + 1 < 4:
                                bg["chunks"] = b_select_chunks(g + 1)
                                nsteps = 4 * (4 * g + 4)
                                bg["per"] = -(-len(bg["chunks"]) // nsteps)
                            b_attend(g)
                            bg_drain()

            if debug and l == 0:
                S.barrier()
                dma("sp", dbg_m, mixT.rearrange("p a b -> p (a b)"), [t for tt_ in T_mix for t in tt_], [T_dbg])

            S.barrier()
            xsrc = x_d if l == 0 else xres_d
            for tb in range(NT):
                dma("sp", X[:, tb, :], xsrc[tb * 128:(tb + 1) * 128, :], [T_xres], [T_X[tb]])
            s0 = w_get()
            s1 = w_get()
            for tb in range(NT):
                for hf, sl in enumerate((s0, s1)):
                    pi = next_ps()
                    for kc in range(8):
                        mm(psb[pi][:, :], mixT[:, kc, tb * 128:(tb + 1) * 128], wunit[sl][:, kc, :], kc == 0, kc == 7,
                           [T_wu[sl], T_mix[kc // 2][tb // 4]], [T_ps[pi]])
                    tt("dve", X[:, tb, hf * 512:(hf + 1) * 512], X[:, tb, hf * 512:(hf + 1) * 512], psb[pi][:, :], ALU.add,
                       [T_ps[pi], T_X[tb]], [T_X[tb]])
            w_release()
            w_release()
            if debug and l == 0 and not do_ffn:
                for tb in range(NT):
                    dma("sp", dbg_x[tb * 128:(tb + 1) * 128, :], X[:, tb, :], [T_X[tb]], [T_dbg])

            if do_ffn:
                norm_to_hT(l, g2_d[l:l + 1, :])
                actT = mixT
                T_act = [Tok() for _ in range(NT)]
                for (f0, nf) in ((0, 1024), (1024, 1024), (2048, 768)):
                    nfc = nf // 128
                    fc = 0
                    for c0 in range(f0, f0 + nf, 512):
                        nn = min(512, f0 + nf - c0)
                        sg = w_get()
                        su = w_get()
                        for ci in range(nn // 128):
                            for tg in range(4):
                                pg = next_ps()
                                pu = next_ps()
                                for kc in range(8):
                                    mm(psb[pg][:, :], wunit[sg][:, kc, ci * 128:(ci + 1) * 128], hT[:, kc, tg * 512:(tg + 1) * 512],
                                       kc == 0, kc == 7, [T_wu[sg]] + T_hT[tg * 4:tg * 4 + 4], [T_ps[pg]])
                                for kc in range(8):
                                    mm(psb[pu][:, :], wunit[su][:, kc, ci * 128:(ci + 1) * 128], hT[:, kc, tg * 512:(tg + 1) * 512],
                                       kc == 0, kc == 7, [T_wu[su]] + T_hT[tg * 4:tg * 4 + 4], [T_ps[pu]])
                                k = rope_rr[0] % 4
                                rope_rr[0] += 1
                                act(tmp[k], psb[pg][:, :], AF.Silu, [T_ps[pg]], [T_tmp[k]])
                                tt("dve", actT[:, fc, tg * 512:(tg + 1) * 512], psb[pu][:, :], tmp[k], ALU.mult,
                                   [T_ps[pu], T_tmp[k]], T_act[tg * 4:tg * 4 + 4])
                            fc += 1
                        w_release()
                        w_release()
                    for hf in range(2):
                        sd = w_get()
                        for tb in range(NT):
                            pi = next_ps()
                            for k2 in range(nfc):
                                mm(psb[pi][:, :], actT[:, k2, tb * 128:(tb + 1) * 128], wunit[sd][:, k2, :], k2 == 0, k2 == nfc - 1,
                                   [T_wu[sd], T_act[tb]], [T_ps[pi]])
                            tt("dve", X[:, tb, hf * 512:(hf + 1) * 512], X[:, tb, hf * 512:(hf + 1) * 512], psb[pi][:, :], ALU.add,
                               [T_ps[pi], T_X[tb]], [T_X[tb]])
                        w_release()
                if debug and l == 0:
                    for tb in range(NT):
                        dma("sp", dbg_x[tb * 128:(tb + 1) * 128, :], X[:, tb, :], [T_X[tb]], [T_dbg])

        if do_final:
            S.barrier()
            dma("sp", G, bcast_rows(gf_d[0:1, :]), [], [T_G])
            T_ob = [Tok(), Tok()]
            for tb in range(NT):
                b = tb % 2
                ssq = small[:, tb:tb + 1]
                std = small[:, 16 + tb:17 + tb]
                rstd = small[:, 32 + tb:33 + tb]
                act(junk, X[:, tb, :], AF.Square, [T_X[tb]], [T_junk, T_ss[tb]], accum_out=ssq)
                act(std, ssq, AF.Sqrt, [T_ss[tb], T_cst], [T_ss[tb]], scale=1.0 / DM, bias=eps_t)
                S.add("dve", lambda e, o=rstd, i=std: e.reciprocal(out=o, in_=i), [T_ss[tb]], [T_ss[tb]])
                stt(obuf[b], X[:, tb, :], rstd, G, ALU.mult, ALU.mult, [T_X[tb], T_ss[tb], T_G], [T_ob[b]])
                dma("sp", out_d[tb * 128:(tb + 1) * 128, :], obuf[b], [T_ob[b]], [T_out])
        else:
            for tb in range(NT):
                dma("sp", out_d[tb * 128:(tb + 1) * 128, :], X[:, tb, :], [T_X[tb]], [T_out])

        stats = S.emit(sems, dsems)
    return nc, stats


def _prep_shared(ln1_g, w_in, rel_bias, forget_b, lam_q1, lam_k1, lam_q2, lam_k2, diff_norm_g, w_o, ln2_g,
                 w_gate, w_up, w_down, final_g):
    f = lambda a: np.ascontiguousarray(np.asarray(a, dtype=np.float32))
    sh = {}
    sh["win"] = f(np.asarray(w_in)[:, :, WIN_COLS])
    sh["wo"] = f(w_o)
    sh["wg"] = f(w_gate)
    sh["wu"] = f(w_up)
    sh["wd"] = f(w_down)
    sh["g1"] = f(ln1_g)
    sh["g2"] = f(ln2_g)
    sh["gf"] = f(np.asarray(final_g).reshape(1, DM))
    rb = np.asarray(rel_bias, dtype=np.float32)
    k = np.arange(128)[:, None]
    q = np.arange(128)[None, :]
    tiles = []
    for dl in range(2):
        idx = np.clip(q - k + 128 * dl, -128, 128) + 128
        tiles.append(rb[:, :, idx])
    t = np.stack(tiles, axis=2)
    sh["relb"] = f(np.transpose(t, (0, 3, 1, 2, 4)).reshape(NL, 128, 4 * 2 * 128))
    sh["relc"] = f(rb[:, :, 256])
    sh["fb"] = f(forget_b)
    sh["lamv"] = f(np.concatenate([np.asarray(lam_q1), np.asarray(lam_k1), np.asarray(lam_q2), np.asarray(lam_k2)], axis=1))
    sh["dng"] = f(diff_norm_g)
    sh["cs64"], sh["ss64"] = _rope_tables(64)
    sh["cs32"], sh["ss32"] = _rope_tables(32)
    return sh


_CACHE = {}


def kernel(x, ln1_g, w_in, rel_bias, forget_b, lam_q1, lam_k1, lam_q2, lam_k2, diff_norm_g, w_o, ln2_g,
           w_gate, w_up, w_down, final_g):
    x = np.asarray(x, dtype=np.float32)
    sh = _prep_shared(ln1_g, w_in, rel_bias, forget_b, lam_q1, lam_k1, lam_q2, lam_k2, diff_norm_g, w_o, ln2_g,
                      w_gate, w_up, w_down, final_g)
    if "nc" not in _CACHE:
        _CACHE["nc"] = build_program()[0]
    nc = _CACHE["nc"]
    in_maps = []
    for c in range(NCORES):
        mp = dict(sh)
        mp["x"] = np.ascontiguousarray(x[c])
        in_maps.append(mp)
    res = run_bass_kernel_spmd(nc, in_maps, core_ids=list(range(NCORES)))
    out = np.stack([np.asarray(res.results[c]["out"], dtype=np.float32).reshape(SEQ, DM) for c in range(NCORES)], axis=0)
    return out
```
